# Optimizing a Trainium2 kernel written in Bass

```python
import jax
import jax.numpy as jnp
from jax import lax
import numpy as np

D_MODEL = 1024
BATCH = 16
SEQ = 4096
DEPTH = 4

CTX_LEN = 256
GRID_W = 64
N_MOD = 6
EPS = 1e-6
MASK_VALUE = -1e30
CONV_W = 512
CONV_K = 3
HG_HEADS = 4
HG_DK = 128
HG_DV = 128
HG_WK = HG_HEADS * HG_DK
HG_WV = HG_HEADS * HG_DV
HG_CHUNK = 64
NA_HEADS = 8
NA_HD = 64
NA_W = NA_HEADS * NA_HD
WIN_R = 8
WIN_C = 16
QB_C = 16
KB_C = WIN_C + QB_C
N_EXPERTS = 16
CAP_FACTOR = 2
F_EXPERT = 2048
IN_WIDTHS = (CONV_W, CONV_W, CONV_W, HG_WK, HG_WV, HG_WK, HG_WK, HG_WV, NA_W, NA_W, NA_W, D_MODEL, D_MODEL, D_MODEL)
N_IN = sum(IN_WIDTHS)
IN_SPLITS = tuple(sum(IN_WIDTHS[:i + 1]) for i in range(len(IN_WIDTHS) - 1))

kernel_name = 'hybrid_conv_hgrn2_natten_ecmoe_dit'


def rmsnorm(x, w):
    xf = x.astype(jnp.float32)
    y = xf * lax.rsqrt(jnp.mean(xf * xf, axis=-1, keepdims=True) + EPS)
    return (y * w.astype(jnp.float32)).astype(x.dtype)


def heads(a, n):
    return a.reshape(a.shape[:-1] + (n, a.shape[-1] // n))


def modulated_norm(x, w, shift, scale):
    return rmsnorm(x, w) * (1 + scale) + shift


def mixer_inputs(x, mod, norm_w, w_in):
    h = modulated_norm(x, norm_w, mod[:, :, 0], mod[:, :, 1])
    return jnp.split(h @ w_in, IN_SPLITS, axis=-1)


def short_conv_mixer(gate_b, gate_c, u, conv_w):
    v = gate_c * u
    y = lax.conv_general_dilated(v, conv_w[:, None, :].astype(v.dtype), window_strides=(1,),
                                 padding=((CONV_K // 2, CONV_K // 2),),
                                 dimension_numbers=('NWC', 'WIO', 'NWC'),
                                 feature_group_count=v.shape[-1])
    return gate_b * y


def _chunks(a):
    b, t, h, d = a.shape
    return a.reshape(b, t // HG_CHUNK, HG_CHUNK, h, d).transpose(1, 0, 3, 2, 4)


def gla_chunk_scan(q, k, v, log_g, s0):
    b_, t_, h_, _ = q.shape
    tri = jnp.tril(jnp.ones((HG_CHUNK, HG_CHUNK), dtype=bool))[:, :, None]

    def step(s, xs):
        qc, kc, vc, gc = xs
        bcum = jnp.cumsum(gc, axis=2)
        o_inter = jnp.einsum('bhtk,bhkv->bhtv', qc * jnp.exp(bcum), s)
        diff = bcum[:, :, :, None, :] - bcum[:, :, None, :, :]
        decay = jnp.where(tri, jnp.exp(jnp.where(tri, diff, 0.0)), 0.0)
        attn = jnp.einsum('bhtk,bhsk,bhtsk->bhts', qc, kc, decay)
        o_intra = jnp.einsum('bhts,bhsv->bhtv', attn, vc)
        b_end = bcum[:, :, -1:, :]
        s_new = jnp.exp(b_end[:, :, 0, :])[..., None] * s + jnp.einsum('bhsk,bhsv->bhkv', kc * jnp.exp(b_end - bcum), vc)
        return s_new, o_inter + o_intra

    s_fin, o = lax.scan(step, s0, (_chunks(q), _chunks(k), _chunks(v), _chunks(log_g)))
    o = o.transpose(1, 0, 3, 2, 4).reshape(b_, t_, h_, -1)
    return o, s_fin


def hgrn2_gates(f, lb):
    f = f.astype(jnp.float32)
    sig = jax.nn.sigmoid(f)
    log_g = jnp.log(lb + (1.0 - lb) * sig)
    k = (1.0 - lb) * (1.0 - sig)
    return heads(k, HG_HEADS), heads(log_g, HG_HEADS)


def hgrn2_direction(q, v, f, qc, vc, fc, lb):
    k, log_g = hgrn2_gates(f, lb)
    kc, log_gc = hgrn2_gates(fc, lb)
    s0 = jnp.zeros((q.shape[0], HG_HEADS, HG_DK, HG_DV), jnp.float32)
    oc, s_ctx = gla_chunk_scan(qc, kc, vc, log_gc, s0)
    o, _ = gla_chunk_scan(q, k, v, log_g, s_ctx)
    return o, oc


def hgrn2_bidirectional(q, i, f_fw, f_bw, qc, ic, fc_fw, fc_bw, lb):
    q, qc = [heads(jax.nn.silu(a.astype(jnp.float32)) * HG_DK ** -0.5, HG_HEADS) for a in (q, qc)]
    v, vc = [heads(a.astype(jnp.float32), HG_HEADS) for a in (i, ic)]
    o_fw, oc_fw = hgrn2_direction(q, v, f_fw, qc, vc, fc_fw, lb[0])
    flip = lambda a: jnp.flip(a, axis=1)
    o_bw, oc_bw = hgrn2_direction(flip(q), flip(v), flip(f_bw), flip(qc), flip(vc), flip(fc_bw), lb[1])
    return o_fw + flip(o_bw), oc_fw + flip(oc_bw)


def hgrn2_readout(o, g, norm_w):
    y = rmsnorm(o, norm_w) * jax.nn.silu(heads(g, HG_HEADS).astype(jnp.float32))
    return y.reshape(y.shape[:2] + (HG_WV,)).astype(g.dtype)


def neighbourhood_attention(q, k, v, kc, vc, rpb):
    b_, s_, h_, hd = q.shape
    rows = s_ // GRID_W
    wr = min(WIN_R, rows)
    ncb = GRID_W // QB_C
    scale = hd ** -0.5
    q = q.reshape(b_, rows, ncb, QB_C, h_, hd).transpose(1, 0, 2, 3, 4, 5)
    k = k.reshape(b_, rows, GRID_W, h_, hd)
    v = v.reshape(b_, rows, GRID_W, h_, hd)
    qcol = jnp.arange(GRID_W).reshape(ncb, QB_C)
    wstart = jnp.clip(qcol - WIN_C // 2, 0, GRID_W - WIN_C)
    kcol = jnp.clip(jnp.arange(ncb) * QB_C - WIN_C // 2, 0, GRID_W - KB_C)[:, None] + jnp.arange(KB_C)
    in_win = (kcol[:, None, :] >= wstart[..., None]) & (kcol[:, None, :] < wstart[..., None] + WIN_C)
    dc = jnp.clip(kcol[:, None, :] - qcol[..., None] + WIN_C - 1, 0, 2 * WIN_C - 2)
    rpb_c = rpb[:, :, dc]
    rstart = jnp.clip(jnp.arange(rows) - WIN_R // 2, 0, rows - wr)

    def row_block(args):
        r, qr = args
        r0 = rstart[r]
        kr = lax.dynamic_slice_in_dim(k, r0, wr, axis=1)[:, :, kcol]
        vr = lax.dynamic_slice_in_dim(v, r0, wr, axis=1)[:, :, kcol]
        dr = r0 + jnp.arange(wr) - r + WIN_R - 1
        bias = rpb_c[:, dr].transpose(0, 2, 3, 1, 4)
        s_loc = jnp.einsum('bnqhd,bwnkhd->bhnqwk', qr, kr).astype(jnp.float32) * scale + bias[None].astype(jnp.float32)
        s_loc = jnp.where(in_win[:, :, None, :], s_loc, MASK_VALUE).reshape(b_, h_, ncb, QB_C, wr * KB_C)
        s_ctx = jnp.einsum('bnqhd,blhd->bhnql', qr, kc).astype(jnp.float32) * scale
        p = jax.nn.softmax(jnp.concatenate([s_loc, s_ctx], axis=-1), axis=-1).astype(v.dtype)
        p_loc = p[..., :wr * KB_C].reshape(b_, h_, ncb, QB_C, wr, KB_C)
        p_ctx = p[..., wr * KB_C:]
        o = jnp.einsum('bhnqwk,bwnkhd->bnqhd', p_loc, vr) + jnp.einsum('bhnql,blhd->bnqhd', p_ctx, vc)
        return o.reshape(b_, GRID_W, h_, hd)

    out = lax.map(row_block, (jnp.arange(rows), q))
    return out.transpose(1, 0, 2, 3, 4).reshape(b_, s_, h_ * hd)


def context_attention(qc, kc, vc):
    s = jnp.einsum('blhd,bmhd->bhlm', qc, kc).astype(jnp.float32) * qc.shape[-1] ** -0.5
    p = jax.nn.softmax(s, axis=-1).astype(vc.dtype)
    o = jnp.einsum('bhlm,bmhd->blhd', p, vc)
    return o.reshape(o.shape[:2] + (-1,))


def merge_branches(y_a, y_b, y_c, g_a, g_b, g_c, w_br_a, w_br_b, w_br_c, w_out):
    m = (jax.nn.sigmoid(g_a) * (y_a @ w_br_a) + jax.nn.sigmoid(g_b) * (y_b @ w_br_b)
         + jax.nn.sigmoid(g_c) * (y_c @ w_br_c))
    return m @ w_out


def expert_choice_ffn(h, w_router, w_e_gate, w_e_up, w_e_down):
    cap = CAP_FACTOR * h.shape[1] // N_EXPERTS

    def route_group(hg):
        aff = jax.nn.softmax((hg @ w_router).astype(jnp.float32), axis=-1)
        wgt, idx = lax.top_k(aff.T, cap)
        xe = hg[idx]
        a = jnp.einsum('ecd,edf->ecf', xe, w_e_gate)
        u = jnp.einsum('ecd,edf->ecf', xe, w_e_up)
        ye = jnp.einsum('ecf,efd->ecd', jax.nn.silu(a) * u, w_e_down) * wgt[..., None].astype(hg.dtype)
        return jnp.zeros_like(hg).at[idx.reshape(-1)].add(ye.reshape(-1, hg.shape[-1]).astype(hg.dtype))

    return lax.map(route_group, h)


def trunk_layer(x, xc, mod, mod_c, w_in, conv_w, lb, hg_norm, q_norm, k_norm, rpb, w_br_a, w_br_b, w_br_c,
                w_out, norm1, norm2, w_router, w_e_gate, w_e_up, w_e_down, last):
    (a_b, a_c, a_u, h_q, h_i, h_ffw, h_fbw, h_g, n_q, n_k, n_v, g_a, g_b, g_c) = mixer_inputs(x, mod, norm1, w_in)
    (ca_b, ca_c, ca_u, ch_q, ch_i, ch_ffw, ch_fbw, ch_g, cn_q, cn_k, cn_v, cg_a, cg_b, cg_c) = mixer_inputs(xc, mod_c, norm1, w_in)
    o_hg, oc_hg = hgrn2_bidirectional(h_q, h_i, h_ffw, h_fbw, ch_q, ch_i, ch_ffw, ch_fbw, lb)
    kc = rmsnorm(heads(cn_k, NA_HEADS), k_norm)
    vc = heads(cn_v, NA_HEADS)
    y_na = neighbourhood_attention(rmsnorm(heads(n_q, NA_HEADS), q_norm), rmsnorm(heads(n_k, NA_HEADS), k_norm),
                                   heads(n_v, NA_HEADS), kc, vc, rpb)
    y_cv = short_conv_mixer(a_b, a_c, a_u, conv_w)
    y_hg = hgrn2_readout(o_hg, h_g, hg_norm)
    x = x + mod[:, :, 2] * merge_branches(y_cv, y_hg, y_na, g_a, g_b, g_c, w_br_a, w_br_b, w_br_c, w_out)
    if not last:
        yc_na = context_attention(rmsnorm(heads(cn_q, NA_HEADS), q_norm), kc, vc)
        yc_cv = short_conv_mixer(ca_b, ca_c, ca_u, conv_w)
        yc_hg = hgrn2_readout(oc_hg, ch_g, hg_norm)
        xc = xc + mod_c[:, :, 2] * merge_branches(yc_cv, yc_hg, yc_na, cg_a, cg_b, cg_c, w_br_a, w_br_b, w_br_c, w_out)
    x = x + mod[:, :, 5] * expert_choice_ffn(modulated_norm(x, norm2, mod[:, :, 3], mod[:, :, 4]),
                                             w_router, w_e_gate, w_e_up, w_e_down)
    if not last:
        xc = xc + mod_c[:, :, 5] * expert_choice_ffn(modulated_norm(xc, norm2, mod_c[:, :, 3], mod_c[:, :, 4]),
                                                     w_router, w_e_gate, w_e_up, w_e_down)
    return x, xc


def setup_inputs(seed: int = 0) -> dict:
    key = jax.random.key(seed)
    ks = jax.random.split(key, 24)
    D = D_MODEL

    def nrm(k, shape, s):
        return jax.random.normal(k, shape, jnp.float32) * s

    return {
        'x': nrm(ks[0], (BATCH, SEQ, D), 1.0),
        'c': nrm(ks[1], (BATCH, D), 1.0),
        'ctx': nrm(ks[2], (BATCH, CTX_LEN, D), 1.0),
        'c_ctx': nrm(ks[3], (D,), 1.0),
        'w_mod': nrm(ks[4], (DEPTH, D, N_MOD * D), 0.5 * D ** -0.5),
        'b_mod': nrm(ks[5], (DEPTH, N_MOD * D), 0.02),
        'norm1': 1.0 + nrm(ks[6], (DEPTH, D), 0.02),
        'w_in': nrm(ks[7], (DEPTH, D, N_IN), D ** -0.5),
        'conv_w': nrm(ks[8], (DEPTH, CONV_K, CONV_W), CONV_K ** -0.5),
        'hg_lb_logits': nrm(ks[9], (DEPTH, 2, HG_WK), 1.0),
        'hg_norm': 1.0 + nrm(ks[10], (DEPTH, HG_DV), 0.02),
        'na_q_norm': 1.0 + nrm(ks[11], (DEPTH, NA_HD), 0.02),
        'na_k_norm': 1.0 + nrm(ks[12], (DEPTH, NA_HD), 0.02),
        'na_rpb': nrm(ks[13], (DEPTH, NA_HEADS, 2 * WIN_R - 1, 2 * WIN_C - 1), 0.02),
        'w_br_a': nrm(ks[14], (DEPTH, CONV_W, D), CONV_W ** -0.5),
        'w_br_b': nrm(ks[15], (DEPTH, HG_WV, D), HG_WV ** -0.5),
        'w_br_c': nrm(ks[16], (DEPTH, NA_W, D), NA_W ** -0.5),
        'w_out': nrm(ks[17], (DEPTH, D, D), D ** -0.5),
        'norm2': 1.0 + nrm(ks[18], (DEPTH, D), 0.02),
        'w_router': nrm(ks[19], (DEPTH, D, N_EXPERTS), D ** -0.5),
        'w_e_gate': nrm(ks[20], (DEPTH, N_EXPERTS, D, F_EXPERT), D ** -0.5),
        'w_e_up': nrm(ks[21], (DEPTH, N_EXPERTS, D, F_EXPERT), D ** -0.5),
        'w_e_down': nrm(ks[22], (DEPTH, N_EXPERTS, F_EXPERT, D), F_EXPERT ** -0.5),
    }


def reference(x, c, ctx, c_ctx, w_mod, b_mod, norm1, w_in, conv_w, hg_lb_logits, hg_norm, na_q_norm, na_k_norm,
              na_rpb, w_br_a, w_br_b, w_br_c, w_out, norm2, w_router, w_e_gate, w_e_up, w_e_down):
    lb_sm = jax.nn.softmax(hg_lb_logits.astype(jnp.float32), axis=0)
    lb_all = jnp.cumsum(lb_sm, axis=0) - lb_sm[0]
    cond = jax.nn.silu(c)
    cond_c = jax.nn.silu(c_ctx)[None]
    xc = ctx
    for l in range(DEPTH):
        mod = (cond @ w_mod[l] + b_mod[l]).reshape(-1, 1, N_MOD, D_MODEL)
        mod_c = (cond_c @ w_mod[l] + b_mod[l]).reshape(1, 1, N_MOD, D_MODEL)
        x, xc = trunk_layer(x, xc, mod, mod_c, w_in[l], conv_w[l], lb_all[l], hg_norm[l], na_q_norm[l],
                            na_k_norm[l], na_rpb[l], w_br_a[l], w_br_b[l], w_br_c[l], w_out[l], norm1[l],
                            norm2[l], w_router[l], w_e_gate[l], w_e_up[l], w_e_down[l], l == DEPTH - 1)
    return x
```

```python
import numpy as np
import ml_dtypes
import concourse.bass as bass
import concourse.mybir as mybir
from concourse.bass_utils import run_bass_kernel_spmd

F32 = mybir.dt.float32
F32R = mybir.dt.float32r
BF16 = mybir.dt.bfloat16
U32 = mybir.dt.uint32
I32 = mybir.dt.int32
AF = mybir.ActivationFunctionType
ALU = mybir.AluOpType
AX = mybir.AxisListType

D = 1024
CTX = 256
NIN = 8704
NE = 16
FE = 2048
EPS = 1e-6
NEG = -1e30


class Res:
    __slots__ = ("w", "r")

    def __init__(self):
        self.w = {}
        self.r = {}


class V:
    __slots__ = ("ap", "res")

    def __init__(self, ap, res):
        self.ap = ap
        self.res = res if isinstance(res, list) else [res]

    def __getitem__(self, idx):
        return V(self.ap[idx], self.res)

    def re(self, pat, **kw):
        return V(self.ap.rearrange(pat, **kw), self.res)

    def bc(self, shape):
        return V(self.ap.to_broadcast(shape), self.res)

    def cast(self, dt):
        return V(self.ap.bitcast(dt), self.res)


OUT_KEYS = ("out", "accum_out", "ap")


class Prog:
    def __init__(self, nc, nds=(("sp", 40), ("pool", 40), ("act", 8))):
        self.nc = nc
        self.engs = {"pe": nc.tensor, "act": nc.scalar, "dve": nc.vector, "pool": nc.gpsimd, "sp": nc.sync}
        self.items = {e: [] for e in self.engs}
        self.semh = {}
        self.cnt = {}
        for e in self.engs:
            self.semh[e] = nc.alloc_semaphore("s_" + e)
            self.cnt[e] = 0
        self.dq = {}
        for q, n in nds:
            lst = []
            for i in range(n):
                k = "d_%s%d" % (q, i)
                self.semh[k] = nc.alloc_semaphore(k)
                self.cnt[k] = 0
                lst.append(k)
            self.dq[q] = [lst, 0]
        self.waited = {e: {} for e in self.engs}
        self.rk = {}
        self.nops = 0

    def R(self, key):
        r = self.rk.get(key)
        if r is None:
            r = self.rk[key] = Res()
        return r

    def _deps(self, eng, reads, writes):
        need = {}
        for r in reads:
            for s, v in r.w.items():
                if need.get(s, 0) < v:
                    need[s] = v
        for w in writes:
            for s, v in w.w.items():
                if need.get(s, 0) < v:
                    need[s] = v
            for s, v in w.r.items():
                if need.get(s, 0) < v:
                    need[s] = v
        waits = []
        wd = self.waited[eng]
        for s, v in need.items():
            if eng == "pe" and s == "pe":
                continue
            if wd.get(s, 0) >= v:
                continue
            wd[s] = v
            waits.append((s, v))
        return waits

    def _mark(self, tok, reads, writes):
        s, v = tok
        for r in reads:
            r.r[s] = v
        for w in writes:
            w.w = {s: v}
            w.r = {}

    def op(self, eng, fn, reads, writes):
        waits = self._deps(eng, reads, writes)
        self.cnt[eng] += 1
        self._mark((eng, self.cnt[eng]), reads, writes)
        self.items[eng].append((waits, fn, (eng, 1)))
        self.nops += 1

    def dma_raw(self, q, fn, reads, writes):
        lst, i = self.dq[q]
        k = lst[i % len(lst)]
        self.dq[q][1] = i + 1
        waits = self._deps(q, reads, writes)
        pv = self.cnt[k]
        if pv > 0 and self.waited[q].get(k, 0) < pv:
            waits.append((k, pv))
            self.waited[q][k] = pv
        self.cnt[k] += 16
        self._mark((k, self.cnt[k]), reads, writes)
        self.items[q].append((waits, fn, (k, 16)))
        self.nops += 1

    def do(self, eng, meth, **kw):
        reads, writes, args = [], [], {}
        for k, v in kw.items():
            if isinstance(v, V):
                args[k] = v.ap
                if k in OUT_KEYS:
                    writes.extend(v.res)
                else:
                    reads.extend(v.res)
            else:
                args[k] = v

        def fn(e, meth=meth, args=args):
            return getattr(e, meth)(**args)

        self.op(eng, fn, reads, writes)

    def dma(self, q, out, in_, rd=(), wr=(), **kw):
        reads = [self.R(k) for k in rd]
        writes = [self.R(k) for k in wr]
        if isinstance(out, V):
            writes.extend(out.res)
            o = out.ap
        else:
            o = out
        if isinstance(in_, V):
            reads.extend(in_.res)
            i = in_.ap
        else:
            i = in_

        def fn(e, o=o, i=i, kw=kw):
            return e.dma_start(out=o, in_=i, **kw)

        self.dma_raw(q, fn, reads, writes)

    def barrier(self):
        for e in self.engs:
            waits = []
            wd = self.waited[e]
            for s, v in self.cnt.items():
                if v > 0 and wd.get(s, 0) < v:
                    wd[s] = v
                    waits.append((s, v))
            if waits:
                self.items[e].append((waits, None, None))

    def emit(self):
        nc = self.nc
        with nc.Block() as block:
            def mk(e):
                def body(eng):
                    for waits, fn, inc in self.items[e]:
                        for (k, v) in waits:
                            eng.wait_ge(self.semh[k], v)
                        if fn is not None:
                            fn(eng).then_inc(self.semh[inc[0]], inc[1])
                return body

            block.tensor(mk("pe"))
            block.scalar(mk("act"))
            block.vector(mk("dve"))
            block.gpsimd(mk("pool"))
            block.sync(mk("sp"))


class Arena:
    def __init__(self, nc, nelem):
        self.t = nc.alloc_sbuf_tensor("arena", [128, nelem], F32)
        self.n = nelem
        self.off = 0

    def reset(self):
        self.off = 0

    def tile(self, shape, dt=F32):
        n = 1
        for s in shape[1:]:
            n *= s
        n32 = n if dt in (F32, U32, I32) else (n + 1) // 2
        n32 = (n32 + 1) // 2 * 2
        assert self.off + n32 <= self.n, ("arena overflow", self.off, n32, self.n)
        ap = self.t[:, self.off:self.off + n32]
        self.off += n32
        if dt != F32:
            ap = ap.bitcast(dt)
            ap = ap[:, 0:n]
        if len(shape) == 3:
            ap = ap.rearrange("p (a b) -> p a b", b=shape[2])
        elif len(shape) == 4:
            ap = ap.rearrange("p (a b c) -> p a b c", b=shape[2], c=shape[3])
        if shape[0] != 128:
            ap = ap[0:shape[0]]
        return V(ap, Res())


def make_groups(S):
    groups = [(0, CTX)]
    for t in range(CTX, CTX + S, 512):
        groups.append((t, min(t + 512, CTX + S)))
    return groups


def build_program(NB, S, DEPTH, dbg=()):
    T = CTX + S
    NS = NB + 1
    NCH = T // 128
    ROWS = S // 64
    CAP = 2 * S // NE
    CAPC = 2 * CTX // NE
    nc = bass.Bass("TRN2", target_bir_lowering=False)
    P = Prog(nc)

    def din(name, shape, dt=F32):
        return nc.dram_tensor(name, list(shape), dt, kind="ExternalInput").ap()

    def dscr(name, shape, dt=F32):
        kind = "ExternalOutput" if name in dbg else "Internal"
        return nc.dram_tensor(name, list(shape), dt, kind=kind).ap()

    x_in = din("x", [NB, S, D])
    c_in = din("c", [NB, D])
    ctx_in = din("ctx", [NB, CTX, D])
    cctx_in = din("c_ctx", [D])
    w_mod = din("w_mod", [DEPTH, D, 6 * D])
    b_mod = din("b_mod", [DEPTH, 6 * D])
    norm1 = din("norm1", [DEPTH, D])
    w_in = din("w_in", [DEPTH, D, NIN])
    conv_w = din("conv_w", [DEPTH, 3, 512])
    lb_log = din("hg_lb_logits", [DEPTH, 2, 512])
    hg_norm = din("hg_norm", [DEPTH, 128])
    q_norm = din("na_q_norm", [DEPTH, 64])
    k_norm = din("na_k_norm", [DEPTH, 64])
    rpbx = din("rpbx", [DEPTH, 8, 128, 15, 64])
    w_br = [din("w_br_a", [DEPTH, 512, D]), din("w_br_b", [DEPTH, 512, D]), din("w_br_c", [DEPTH, 512, D])]
    w_out = din("w_out", [DEPTH, D, D])
    norm2 = din("norm2", [DEPTH, D])
    w_router = din("w_router", [DEPTH, D, NE])
    w_eg = din("w_e_gate", [DEPTH, NE, D, FE])
    w_eu = din("w_e_up", [DEPTH, NE, D, FE])
    w_ed = din("w_e_down", [DEPTH, NE, FE, D])
    cst_in = din("consts", [7, 128, 128])
    y_out = nc.dram_tensor("y", [NB, S, D], F32, kind="ExternalOutput").ap()

    X = dscr("X", [NB, T, D])
    H2 = dscr("H2", [NB * T, D])
    PF = [dscr("PF%d" % b_, [NIN, T]) for b_ in range(NB)]
    PK = dscr("PK", [NB, 2, T, 512])
    Y = dscr("Y", [NB, 3, 512, T])
    modrow_d = dscr("modrow", [NS, 6 * D])
    idx_d = dscr("idx_d", [64, CAP], U32)
    wgt_d = dscr("wgt_d", [64, CAP])
    idxc_d = dscr("idxc_d", [64, CAPC], U32)
    wgtc_d = dscr("wgtc_d", [64, CAPC])

    DBG = dscr("DBG", [12, 128, T + 2]) if "DBG" in dbg else None

    def dump(i, v, n):
        if DBG is not None:
            P.dma("sp", DBG[i, :, 0:n], v)

    def sb(name, shape, dt=F32):
        t = nc.alloc_sbuf_tensor(name, list(shape), dt)
        return V(t.ap(), Res())

    cst = sb("cst", [128, 7, 128])
    ident = cst[:, 0, :]
    triU = cst[:, 1, :]
    triL = cst[:, 2, :]
    blkones = cst[:, 3, :]
    ones = cst[:, 4, :]
    maskadd = cst[:, 5, 0:64]
    onesb = sb("onesb", [128, 64], BF16)
    condT = sb("condT", [128, 8, NS])
    modT = sb("modT", [128, 48, NS])
    A1T = sb("A1T", [128, 8, NS])
    n1T = sb("n1T", [128, 8])
    lbT = sb("lbT", [128, DEPTH, 8])
    omlT = sb("omlT", [128, DEPTH, 8])
    nomlT = sb("nomlT", [128, DEPTH, 8])
    lbtmp = sb("lbtmp", [128, 8])
    cwT = sb("cwT", [128, 4, 3])
    hgw = sb("hgw", [128, 1])
    qkw = sb("qkw", [128, 2])
    SMALL = sb("small", [128, 64])

    psum = [V(nc.alloc_psum_tensor("ps%d" % i, [128, 512], F32).ap(), Res()) for i in range(8)]
    psi = [0]

    def ps():
        p = psum[psi[0] % 8]
        psi[0] += 1
        return p

    remaining = nc.sbuf_bytes_remaining
    print("sbuf remaining", remaining)
    arena_elems = (remaining - 2048) // 4
    A = Arena(nc, arena_elems)

    def mm(out, lhsT, rhs, start=True, stop=True, r=True):
        if r:
            lhsT = lhsT.cast(F32R)
            rhs = rhs.cast(F32R)
        P.do("pe", "matmul", out=out, lhsT=lhsT, rhs=rhs, start=start, stop=stop)

    evi = [0]

    def evac(out, in_):
        evi[0] += 1
        if evi[0] % 2:
            P.do("act", "activation", out=out, in_=in_, func=AF.Copy)
        else:
            P.do("dve", "tensor_copy", out=out, in_=in_)

    groups = make_groups(S)
    SBMAX = 2304
    sblocks = []
    cur = []
    for g in groups:
        if cur and (g[1] - cur[0][0]) > SBMAX:
            sblocks.append(cur)
            cur = []
        cur.append(g)
    if cur:
        sblocks.append(cur)

    def pfkeys(b, row0, nrows):
        ks = []
        for rr in range(row0 // 128, (row0 + nrows) // 128):
            for gi in range(len(groups)):
                ks.append(("PF", b, rr, gi))
        return ks

    P.dma("sp", cst, cst_in.rearrange("c p f -> p c f"))
    P.do("dve", "memset", ap=onesb, constant=1.0)
    for s in range(NB):
        P.dma("sp", condT[:, :, s], c_in[s].rearrange("(k p) -> p k", p=128), allow_slow_non_contiguous=True)
    P.dma("sp", condT[:, :, NB], cctx_in.rearrange("(k p) -> p k", p=128), allow_slow_non_contiguous=True)
    P.do("act", "activation", out=condT, in_=condT, func=AF.Silu)
    for l_ in range(DEPTH):
        for d_ in range(2):
            P.dma("sp", lbT[:, l_, d_ * 4:(d_ + 1) * 4], lb_log[l_, d_].rearrange("(h p) -> p h", p=128), allow_slow_non_contiguous=True)
    P.do("act", "activation", out=lbT, in_=lbT, func=AF.Exp)
    P.do("dve", "tensor_copy", out=lbtmp, in_=lbT[:, 0, :])
    for l in range(1, DEPTH):
        P.do("dve", "tensor_tensor", out=lbtmp, in0=lbtmp, in1=lbT[:, l, :], op=ALU.add)
    P.do("dve", "reciprocal", out=lbtmp, in_=lbtmp)
    for l in range(DEPTH):
        P.do("dve", "tensor_tensor", out=lbT[:, l, :], in0=lbT[:, l, :], in1=lbtmp, op=ALU.mult)
    P.do("dve", "memset", ap=lbT[:, 0, :], constant=0.0)
    for l in range(2, DEPTH):
        P.do("dve", "tensor_tensor", out=lbT[:, l, :], in0=lbT[:, l, :], in1=lbT[:, l - 1, :], op=ALU.add)
    P.do("dve", "tensor_scalar", out=omlT, in0=lbT, scalar1=-1.0, scalar2=1.0, op0=ALU.mult, op1=ALU.add)
    P.do("dve", "tensor_scalar", out=nomlT, in0=omlT, scalar1=-1.0, scalar2=None, op0=ALU.mult)
    for b in range(NB):
        P.dma("sp", X[b, 0:CTX, :], ctx_in[b], wr=[("X", b, j) for j in range(CTX // 128)])
        for j in range(S // 512):
            P.dma("sp", X[b, CTX + j * 512:CTX + (j + 1) * 512, :], x_in[b, j * 512:(j + 1) * 512, :],
                  wr=[("X", b, CTX // 128 + 4 * j + i) for i in range(4)])

    def slot_of(b, tok):
        return NB if tok < CTX else b

    def phase_mod(l):
        A.reset()
        wm = [A.tile([128, 8, 512]) for _ in range(2)]
        bmod_sb = A.tile([1, 6 * D])
        modrow_sb = A.tile([NS, 6 * D])
        P.dma("sp", bmod_sb, b_mod[l:l + 1, :])
        for cg in range(12):
            w = wm[cg % 2]
            P.dma("sp", w, w_mod[l, :, cg * 512:(cg + 1) * 512].rearrange("(k p) c -> p k c", p=128))
            pt = ps()
            for k in range(8):
                mm(pt[0:NS, :], condT[:, k, :], w[:, k, :], start=(k == 0), stop=False, r=False)
            mm(pt[0:NS, :], ones[0:1, 0:NS], bmod_sb[0:1, cg * 512:(cg + 1) * 512], start=False, stop=True, r=False)
            evac(modrow_sb[0:NS, cg * 512:(cg + 1) * 512], pt[0:NS, :])
        P.dma("sp", modrow_d, modrow_sb, wr=[("modrow",)])
        for s_ in range(NS):
            P.dma("sp", modT[:, :, s_], modrow_d[s_].rearrange("(j p) -> p j", p=128), rd=[("modrow",)], allow_slow_non_contiguous=True)
        P.dma("sp", n1T, norm1[l].rearrange("(k p) -> p k", p=128), allow_slow_non_contiguous=True)
        P.do("dve", "tensor_scalar", out=A1T, in0=modT[:, 8:16, :], scalar1=1.0, scalar2=None, op0=ALU.add)
        P.do("dve", "tensor_tensor", out=A1T, in0=A1T, in1=n1T.re("p (k o) -> p k o", o=1).bc([128, 8, NS]), op=ALU.mult)
        for k_ in range(3):
            P.dma("sp", cwT[:, :, k_], conv_w[l, k_].rearrange("(j p) -> p j", p=128), allow_slow_non_contiguous=True)
        P.dma("sp", hgw, hg_norm[l].rearrange("(p o) -> p o", o=1), allow_slow_non_contiguous=True)
        for hh in range(2):
            P.dma("sp", qkw[hh * 64:(hh + 1) * 64, 0:1], q_norm[l].rearrange("(p o) -> p o", o=1), allow_slow_non_contiguous=True)
            P.dma("sp", qkw[hh * 64:(hh + 1) * 64, 1:2], k_norm[l].rearrange("(p o) -> p o", o=1), allow_slow_non_contiguous=True)
        P.do("dve", "tensor_scalar", out=qkw[:, 0:1], in0=qkw[:, 0:1], scalar1=0.125, scalar2=None, op0=ALU.mult)
        P.barrier()

    def rms_tile(xt, junk, ss, rstd, inv_n):
        P.do("act", "activation", out=junk, in_=xt, func=AF.Square, accum_out=ss)
        P.do("dve", "tensor_scalar", out=rstd, in0=ss, scalar1=inv_n, scalar2=EPS, op0=ALU.mult, op1=ALU.add)
        P.do("act", "activation", out=rstd, in_=rstd, func=AF.Sqrt)
        P.do("dve", "reciprocal", out=rstd, in_=rstd)

    def phase_proj(l, b):
        A.reset()
        hT = A.tile([128, 8, SBMAX], BF16)
        wst = [A.tile([128, 8, 512]) for _ in range(2)]
        wb = [A.tile([128, 8, 512], BF16) for _ in range(2)]
        xt = [A.tile([128, 1024]) for _ in range(4)]
        junk = A.tile([128, 1024])
        st = [A.tile([128, 512]) for _ in range(4)]
        sm = [A.tile([128, 2]) for _ in range(4)]
        sti = [0]
        seq = [(si, cg) for si in range(len(sblocks)) for cg in range(17)]

        def wload(i):
            si, cg = seq[i]
            P.dma("sp", wst[i % 2], w_in[l, :, cg * 512:(cg + 1) * 512].rearrange("(k p) c -> p k c", p=128))
            P.do("pool", "tensor_copy", out=wb[i % 2], in_=wst[i % 2])

        def nmt(sbl):
            t0 = sbl[0][0]
            for (g0, g1) in sbl:
                nt = (g1 - g0) // 128
                slot = slot_of(b, g0)
                for tt in range(nt):
                    tok = g0 + tt * 128
                    P.dma("sp", xt[tt], X[b, tok:tok + 128, :], rd=[("X", b, tok // 128)])
                    rms_tile(xt[tt], junk, sm[tt][:, 0:1], sm[tt][:, 1:2], 1.0 / D)
                    P.do("act", "activation", out=xt[tt], in_=xt[tt], func=AF.Copy, scale=sm[tt][:, 1:2])
                for k in range(8):
                    pt = ps()
                    for tt in range(nt):
                        P.do("pe", "transpose", out=pt[:, tt * 128:(tt + 1) * 128], in_=xt[tt][:, k * 128:(k + 1) * 128], identity=ident)
                    P.do("act", "activation", out=hT[:, k, g0 - t0:g1 - t0], in_=pt[:, 0:nt * 128], func=AF.Identity,
                         scale=A1T[:, k, slot:slot + 1], bias=modT[:, k, slot:slot + 1])

        wload(0)
        for i, (si, cg) in enumerate(seq):
            sbl = sblocks[si]
            t0 = sbl[0][0]
            if cg == 0:
                nmt(sbl)
            if i + 1 < len(seq):
                wload(i + 1)
            w = wb[i % 2]
            if cg in (4, 10):
                which = 0 if cg == 4 else 1
                for (g0, g1) in sbl:
                    for tok in range(g0, g1, 128):
                        pt = ps()
                        for k in range(8):
                            mm(pt, hT[:, k, tok - t0:tok - t0 + 128], w[:, k, :], start=(k == 0), stop=(k == 7), r=False)
                        s_ = st[sti[0] % 4]
                        sti[0] += 1
                        evac(s_, pt)
                        P.dma("sp", PK[b, which, tok:tok + 128, :], s_, wr=[("PK", b, which, tok // 128)])
            else:
                for cc in range(4):
                    for gi, (g0, g1) in enumerate(groups):
                        if (g0, g1) not in sbl:
                            continue
                        n = g1 - g0
                        pt = ps()
                        for k in range(8):
                            mm(pt[:, 0:n], w[:, k, cc * 128:(cc + 1) * 128], hT[:, k, g0 - t0:g1 - t0], start=(k == 0), stop=(k == 7), r=False)
                        s_ = st[sti[0] % 4]
                        sti[0] += 1
                        evac(s_[:, 0:n], pt[:, 0:n])
                        r0 = cg * 512 + cc * 128
                        P.dma("sp", PF[b][r0:r0 + 128, g0:g1], s_[:, 0:n], wr=[("PF", b, r0 // 128, gi)])
        P.barrier()

    def phase_conv(l, b):
        A.reset()
        tb = [[A.tile([128, T]) for _ in range(3)] for _ in range(2)]
        yv = [A.tile([128, T]) for _ in range(2)]
        segs = [(0, CTX), (CTX, T)]
        for j in range(4):
            Bt, Ct, Ut = tb[j % 2]
            y = yv[j % 2]
            for i, t_ in enumerate((Bt, Ct, Ut)):
                r0 = i * 512 + j * 128
                P.dma("sp", t_, PF[b][r0:r0 + 128, :], rd=pfkeys(b, r0, 128))
            P.do("dve", "tensor_tensor", out=Ct, in0=Ct, in1=Ut, op=ALU.mult)
            P.do("dve", "tensor_scalar", out=y, in0=Ct, scalar1=cwT[:, j, 1:2], scalar2=None, op0=ALU.mult)
            for (a, e) in segs:
                P.do("dve", "scalar_tensor_tensor", out=y[:, a + 1:e], in0=Ct[:, a:e - 1], scalar=cwT[:, j, 0:1],
                     in1=y[:, a + 1:e], op0=ALU.mult, op1=ALU.add)
                P.do("dve", "scalar_tensor_tensor", out=y[:, a:e - 1], in0=Ct[:, a + 1:e], scalar=cwT[:, j, 2:3],
                     in1=y[:, a:e - 1], op0=ALU.mult, op1=ALU.add)
            P.do("dve", "tensor_tensor", out=y, in0=y, in1=Bt, op=ALU.mult)
            P.dma("pool", Y[b, 0, j * 128:(j + 1) * 128, :], y, wr=[("Y", b, 0, j)])
        P.barrier()

    DKS = 128 ** -0.5

    def phase_hgrn(l, b):
        A.reset()
        HC = 64
        NC = T // HC
        q = A.tile([128, T])
        kk = A.tile([128, T])
        b2 = A.tile([128, T])
        G = A.tile([128, T + 2])
        qt = A.tile([128, T])
        qh = A.tile([128, T])
        kb = A.tile([128, T])
        Vt = A.tile([HC, NC, 128])
        O = A.tile([128, T])
        Sst = A.tile([128, 128])
        AmD = [[A.tile([HC, HC]) for _ in range(2)] for _ in range(2)]
        qb = A.tile([128, NC, 32])
        kb2 = A.tile([128, NC, 32])
        for d_ in range(2):
            for a_ in AmD[d_]:
                P.do("dve", "memset", ap=a_, constant=0.0)
        kbT = [A.tile([HC, 128]) for _ in range(2)]
        eend = A.tile([128, NC])
        P.do("dve", "memset", ap=G[:, 0:1], constant=0.0)

        def c3(v):
            return v.re("p (c t) -> p c t", t=HC)

        def c32(v):
            return v.re("p (c t) -> p c t", t=32)

        for h in range(4):
            P.dma("sp", q, PF[b][1536 + h * 128:1536 + (h + 1) * 128, :], rd=pfkeys(b, 1536 + h * 128, 128))
            P.do("act", "activation", out=q, in_=q, func=AF.Silu)
            P.dma("sp", Vt, PK[b, 0, :, h * 128:(h + 1) * 128].rearrange("(c p) d -> p c d", p=HC),
                  rd=[("PK", b, 0, i) for i in range(NCH)])
            for d in range(2):
                ix = d * 4 + h
                r0 = 2560 + d * 512 + h * 128
                P.dma("sp", kk, PF[b][r0:r0 + 128, :], rd=pfkeys(b, r0, 128))
                P.do("act", "activation", out=kk, in_=kk, func=AF.Sigmoid)
                P.do("dve", "tensor_scalar", out=b2, in0=kk, scalar1=omlT[:, l, ix:ix + 1], scalar2=lbT[:, l, ix:ix + 1],
                     op0=ALU.mult, op1=ALU.add)
                P.do("act", "activation", out=b2, in_=b2, func=AF.Ln)
                P.do("dve", "tensor_scalar", out=kk, in0=kk, scalar1=nomlT[:, l, ix:ix + 1], scalar2=omlT[:, l, ix:ix + 1],
                     op0=ALU.mult, op1=ALU.add)
                P.do("dve", "tensor_tensor_scan", out=G[:, 1:T + 1], data0=ones[:, 0:1].bc([128, T]), data1=b2, initial=0.0,
                     op0=ALU.mult, op1=ALU.add)
                Gi = c3(G[:, 1:T + 1])
                Hx = c3(G[:, 0:T])
                A3 = Gi if d == 0 else Hx
                Gs = Hx[:, :, 0:1]
                Ge = Gi[:, :, HC - 1:HC]
                A32 = c32(G[:, 1:T + 1]) if d == 0 else c32(G[:, 0:T])
                mid = A32[:, :, 15:16]
                sg = 1.0 if d == 0 else -1.0
                P.do("dve", "tensor_tensor", out=eend.re("p (c o) -> p c o", o=1), in0=Ge, in1=Gs, op=ALU.subtract)
                P.do("act", "activation", out=eend, in_=eend, func=AF.Exp)
                P.do("dve", "tensor_tensor", out=c32(b2), in0=A32, in1=mid.bc([128, T // 32, 32]), op=ALU.subtract)
                P.do("act", "activation", out=qt, in_=b2, func=AF.Exp, scale=sg)
                P.do("dve", "scalar_tensor_tensor", out=qt, in0=q, scalar=DKS, in1=qt, op0=ALU.mult, op1=ALU.mult)
                P.do("act", "activation", out=b2, in_=b2, func=AF.Exp, scale=-sg)
                P.do("dve", "tensor_tensor", out=b2, in0=b2, in1=kk, op=ALU.mult)
                if d == 0:
                    P.do("dve", "tensor_tensor", out=c3(qh), in0=Gi, in1=Gs.bc([128, NC, HC]), op=ALU.subtract)
                    P.do("dve", "tensor_tensor", out=c3(kb), in0=Ge.bc([128, NC, HC]), in1=Gi, op=ALU.subtract)
                else:
                    P.do("dve", "tensor_tensor", out=c3(qh), in0=Ge.bc([128, NC, HC]), in1=Hx, op=ALU.subtract)
                    P.do("dve", "tensor_tensor", out=c3(kb), in0=Hx, in1=Gs.bc([128, NC, HC]), op=ALU.subtract)
                P.do("act", "activation", out=qh, in_=qh, func=AF.Exp)
                P.do("dve", "scalar_tensor_tensor", out=qh, in0=q, scalar=DKS, in1=qh, op0=ALU.mult, op1=ALU.mult)
                P.do("act", "activation", out=kb, in_=kb, func=AF.Exp)
                P.do("dve", "tensor_tensor", out=kb, in0=kb, in1=kk, op=ALU.mult)
                if h == 0 and d == 0:
                    dump(0, q, T); dump(1, kk, T); dump(2, G, T + 2); dump(3, qt, T); dump(4, b2, T); dump(5, qh, T); dump(6, kb, T)
                    dump(7, eend, NC)
                q3 = c3(q)
                k3 = c3(kk)
                if d == 0:
                    Bd = Gi[:, :, 31:32].bc([128, NC, 32])
                    P.do("dve", "tensor_tensor", out=qb, in0=Gi[:, :, 32:64], in1=Bd, op=ALU.subtract)
                    P.do("dve", "tensor_tensor", out=kb2, in0=Bd, in1=Gi[:, :, 0:32], op=ALU.subtract)
                    qsl, ksl = q3[:, :, 32:64], k3[:, :, 0:32]
                else:
                    Bd = Hx[:, :, 32:33].bc([128, NC, 32])
                    P.do("dve", "tensor_tensor", out=qb, in0=Bd, in1=Hx[:, :, 0:32], op=ALU.subtract)
                    P.do("dve", "tensor_tensor", out=kb2, in0=Hx[:, :, 32:64], in1=Bd, op=ALU.subtract)
                    qsl, ksl = q3[:, :, 0:32], k3[:, :, 32:64]
                P.do("act", "activation", out=qb, in_=qb, func=AF.Exp)
                P.do("dve", "scalar_tensor_tensor", out=qb, in0=qsl, scalar=DKS, in1=qb, op0=ALU.mult, op1=ALU.mult)
                P.do("act", "activation", out=kb2, in_=kb2, func=AF.Exp)
                P.do("dve", "tensor_tensor", out=kb2, in0=kb2, in1=ksl, op=ALU.mult)
                P.do("dve", "memset", ap=Sst, constant=0.0)
                ncx = CTX // HC
                order = list(range(NC)) if d == 0 else list(range(ncx - 1, -1, -1)) + list(range(NC - 1, ncx - 1, -1))
                tri = triU if d == 0 else triL
                for ci, c in enumerate(order):
                    cs = slice(c * HC, (c + 1) * HC)
                    pa = ps()
                    c0 = c * HC
                    mm(pa[0:32, 0:32], b2[:, c0:c0 + 32], qt[:, c0:c0 + 32], r=False)
                    mm(pa[32:64, 32:64], b2[:, c0 + 32:c0 + 64], qt[:, c0 + 32:c0 + 64], r=False)
                    ob = (slice(0, 32), slice(32, 64)) if d == 0 else (slice(32, 64), slice(0, 32))
                    mm(pa[ob[0], ob[1]], kb2[:, c, :], qb[:, c, :], r=False)
                    am = AmD[d][ci % 2]
                    P.do("dve", "tensor_tensor", out=am[0:32, 0:32], in0=pa[0:32, 0:32], in1=tri[0:32, 0:32], op=ALU.mult)
                    P.do("dve", "tensor_tensor", out=am[32:64, 32:64], in0=pa[32:64, 32:64], in1=tri[32:64, 32:64], op=ALU.mult)
                    P.do("act", "activation", out=am[ob[0], ob[1]], in_=pa[ob[0], ob[1]], func=AF.Copy)
                    po = ps()
                    mm(po[:, 0:HC], Sst, qh[:, cs], start=True, stop=False, r=False)
                    mm(po[:, 0:HC], Vt[:, c, :], am, start=False, stop=True, r=False)
                    if d == 0:
                        P.do("act", "activation", out=O[:, cs], in_=po[:, 0:HC], func=AF.Copy)
                    else:
                        P.do("dve", "tensor_tensor", out=O[:, cs], in0=O[:, cs], in1=po[:, 0:HC], op=ALU.add)
                    ptr = ps()
                    P.do("pe", "transpose", out=ptr[0:HC, 0:128], in_=kb[:, cs], identity=ident)
                    kt_ = kbT[ci % 2]
                    P.do("act", "activation", out=kt_, in_=ptr[0:HC, 0:128], func=AF.Copy)
                    pS = ps()
                    mm(pS[:, 0:128], kt_, Vt[:, c, :], r=False)
                    P.do("dve", "scalar_tensor_tensor", out=Sst, in0=Sst, scalar=eend[:, c:c + 1], in1=pS[:, 0:128],
                         op0=ALU.mult, op1=ALU.add)
            if h == 0:
                dump(8, O, T)
            P.do("act", "activation", out=b2, in_=O, func=AF.Square)
            for (g0, g1) in groups:
                pt = ps()
                mm(pt[:, 0:g1 - g0], ones, b2[:, g0:g1], r=False)
                P.do("dve", "tensor_scalar", out=qt[:, g0:g1], in0=pt[:, 0:g1 - g0], scalar1=1.0 / 128, scalar2=EPS,
                     op0=ALU.mult, op1=ALU.add)
            P.do("act", "activation", out=qt, in_=qt, func=AF.Sqrt)
            P.do("dve", "reciprocal", out=qt, in_=qt)
            P.do("dve", "tensor_tensor", out=O, in0=O, in1=qt, op=ALU.mult)
            r0 = 3584 + h * 128
            P.dma("sp", kk, PF[b][r0:r0 + 128, :], rd=pfkeys(b, r0, 128))
            P.do("act", "activation", out=kk, in_=kk, func=AF.Silu)
            P.do("dve", "scalar_tensor_tensor", out=O, in0=O, scalar=hgw[:, 0:1], in1=kk, op0=ALU.mult, op1=ALU.mult)
            P.dma("sp", Y[b, 1, h * 128:(h + 1) * 128, :], O, wr=[("Y", b, 1, h)])
        P.barrier()

    def phase_na(l, b):
        NT = S // 128
        for hp in range(4):
            A.reset()
            Q = A.tile([128, T])
            Kt = A.tile([128, T])
            tmp = A.tile([128, T])
            YC = A.tile([128, T])
            Vst = A.tile([128, NT, 128])
            Ve = A.tile([128, NT, 128], BF16)
            Vo = A.tile([128, NT, 128], BF16)
            Vc = A.tile([128, 2, 128], BF16)
            bias = [A.tile([128, 15, 64]) for _ in range(2)]
            PT = [A.tile([128, 512], BF16) for _ in range(2)]
            rden = [A.tile([128, 256]) for _ in range(2)]
            for (tl, r0, wcol) in ((Q, 4096, 0), (Kt, 4608, 1)):
                P.dma("sp", tl, PF[b][r0 + hp * 128:r0 + (hp + 1) * 128, :], rd=pfkeys(b, r0 + hp * 128, 128))
                P.do("act", "activation", out=tmp, in_=tl, func=AF.Square)
                for (g0, g1) in groups:
                    pt = ps()
                    mm(pt[:, 0:g1 - g0], blkones, tmp[:, g0:g1], r=False)
                    P.do("dve", "tensor_scalar", out=tmp[:, g0:g1], in0=pt[:, 0:g1 - g0], scalar1=1.0 / 64, scalar2=EPS,
                         op0=ALU.mult, op1=ALU.add)
                P.do("act", "activation", out=tmp, in_=tmp, func=AF.Sqrt)
                P.do("dve", "reciprocal", out=tmp, in_=tmp)
                P.do("dve", "scalar_tensor_tensor", out=tl, in0=tl, scalar=qkw[:, wcol:wcol + 1], in1=tmp, op0=ALU.mult, op1=ALU.mult)
            pkk = [("PK", b, 1, i) for i in range(NCH)]
            P.dma("sp", Vst, PK[b, 1, CTX:T, hp * 128:(hp + 1) * 128].rearrange("(i p) d -> p i d", p=128), rd=pkk)
            P.do("dve", "tensor_copy", out=Ve, in_=Vst)
            P.dma("sp", Vst[:, 0:NT - 1, :], PK[b, 1, CTX + 64:T - 64, hp * 128:(hp + 1) * 128].rearrange("(i p) d -> p i d", p=128), rd=pkk)
            P.do("dve", "tensor_copy", out=Vo[:, 0:NT - 1, :], in_=Vst[:, 0:NT - 1, :])
            P.dma("sp", Vst[:, 0:2, :], PK[b, 1, 0:CTX, hp * 128:(hp + 1) * 128].rearrange("(i p) d -> p i d", p=128), rd=pkk)
            P.do("dve", "tensor_copy", out=Vc, in_=Vst[:, 0:2, :])
            for hh in range(2):
                P.dma("sp", bias[hh], rpbx[l, 2 * hp + hh])
                P.do("dve", "tensor_tensor", out=bias[hh], in0=bias[hh], in1=maskadd.re("p (o q) -> p o q", o=1).bc([128, 15, 64]), op=ALU.add)
            cnt = [0]

            def attend(qc0, nq, chunks, hh):
                hs = slice(hh * 64, (hh + 1) * 64)
                pt = ps()
                for ci, (ktok, bia, vt) in enumerate(chunks):
                    o_ = pt[:, ci * nq:(ci + 1) * nq]
                    mm(o_, Kt[hs, ktok:ktok + 128], Q[hs, qc0:qc0 + nq], start=True, stop=(bia is None), r=False)
                    if bia is not None:
                        mm(o_, ident, bia, start=False, stop=True, r=False)
                nc_ = len(chunks) * nq
                p_ = PT[cnt[0] % 2]
                rd_ = rden[cnt[0] % 2]
                cnt[0] += 1
                P.do("act", "activation", out=p_[:, 0:nc_], in_=pt[:, 0:nc_], func=AF.Exp)
                po = ps()
                for ci, (ktok, bia, vt) in enumerate(chunks):
                    mm(po[hs, 0:nq], vt, p_[:, ci * nq:(ci + 1) * nq], start=(ci == 0), stop=(ci == len(chunks) - 1), r=False)
                for ci, (ktok, bia, vt) in enumerate(chunks):
                    mm(po[hs, nq:2 * nq], onesb, p_[:, ci * nq:(ci + 1) * nq], start=(ci == 0), stop=(ci == len(chunks) - 1), r=False)
                P.do("dve", "reciprocal", out=rd_[hs, 0:nq], in_=po[hs, nq:2 * nq])
                P.do("dve", "tensor_tensor", out=YC[hs, qc0:qc0 + nq], in0=po[hs, 0:nq], in1=rd_[hs, 0:nq], op=ALU.mult)

            for hh in range(2):
                vs = slice(hh * 64, (hh + 1) * 64)
                cch = [(0, None, Vc[:, 0, vs]), (128, None, Vc[:, 1, vs])]
                attend(0, CTX, cch, hh)
                for r in range(ROWS):
                    r0 = min(max(r - 4, 0), ROWS - 8)
                    dr0 = r0 - r + 7
                    chunks = []
                    for c in range(4):
                        rho = r0 + 2 * c
                        vt = Ve[:, rho // 2, vs] if rho % 2 == 0 else Vo[:, (rho - 1) // 2, vs]
                        chunks.append((CTX + 64 * rho, bias[hh][:, dr0 + 2 * c, :], vt))
                    attend(CTX + 64 * r, 64, chunks + cch, hh)
            P.dma("sp", Y[b, 2, hp * 128:(hp + 1) * 128, :], YC, wr=[("Y", b, 2, hp)])
            P.barrier()

    def bcast_row(tile_, slot, j):
        P.dma("sp", tile_, modrow_d[slot:slot + 1, j * D:(j + 1) * D].to_broadcast([128, D]), rd=[("modrow",)])

    def phase_merge(l, b):
        A.reset()
        wbr = [A.tile([128, 4, 1024], BF16) for _ in range(3)]
        wo = A.tile([128, 8, 1024], BF16)
        wst = [A.tile([128, 4, 1024]) for _ in range(2)]
        yst = [A.tile([128, 4, 512]) for _ in range(2)]
        yb = [A.tile([128, 4, 512], BF16) for _ in range(3)]
        mT = A.tile([128, 8, 512], BF16)
        gst = [A.tile([128, 3, 512]) for _ in range(2)]
        macc = A.tile([128, 512])
        mtmp = A.tile([128, 512])
        xt = [A.tile([128, 1024]) for _ in range(2)]
        otmp = [A.tile([128, 512]) for _ in range(2)]
        g1B = {}
        for slot in (b, NB):
            g1B[slot] = A.tile([128, 1024])
            bcast_row(g1B[slot], slot, 2)
        wi = 0
        for i in range(3):
            P.dma("sp", wst[wi % 2], w_br[i][l].rearrange("(k p) c -> p k c", p=128))
            P.do("pool", "tensor_copy", out=wbr[i], in_=wst[wi % 2])
            wi += 1
        for hf in range(2):
            P.dma("sp", wst[wi % 2], w_out[l, hf * 512:(hf + 1) * 512, :].rearrange("(k p) c -> p k c", p=128))
            P.do("pool", "tensor_copy", out=wo[:, hf * 4:(hf + 1) * 4, :], in_=wst[wi % 2])
            wi += 1
        yi = 0
        gi_ = 0
        xi = 0
        for gi, (g0, g1) in enumerate(groups):
            n = g1 - g0
            slot = slot_of(b, g0)
            for i in range(3):
                ys = yst[yi % 2]
                yi += 1
                P.dma("sp", ys[:, :, 0:n], Y[b, i, :, g0:g1].rearrange("(k p) t -> p k t", p=128), rd=[("Y", b, i, k) for k in range(4)])
                if i == 1:
                    P.do("act", "activation", out=yb[i][:, :, 0:n], in_=ys[:, :, 0:n], func=AF.Copy)
                else:
                    P.do("pool", "tensor_copy", out=yb[i][:, :, 0:n], in_=ys[:, :, 0:n])
            for j in range(8):
                gs = gst[gi_ % 2]
                gi_ += 1
                for i in range(3):
                    r0 = 5632 + i * 1024 + j * 128
                    P.dma("sp", gs[:, i, 0:n], PF[b][r0:r0 + 128, g0:g1], rd=[("PF", b, r0 // 128, gi)])
                P.do("act", "activation", out=gs[:, :, 0:n], in_=gs[:, :, 0:n], func=AF.Sigmoid)
                pts = []
                for i in range(3):
                    pt = ps()
                    for k in range(4):
                        mm(pt[:, 0:n], wbr[i][:, k, j * 128:(j + 1) * 128], yb[i][:, k, 0:n], start=(k == 0), stop=(k == 3), r=False)
                    pts.append(pt)
                P.do("dve", "tensor_tensor", out=macc[:, 0:n], in0=gs[:, 0, 0:n], in1=pts[0][:, 0:n], op=ALU.mult)
                P.do("dve", "tensor_tensor", out=mtmp[:, 0:n], in0=gs[:, 1, 0:n], in1=pts[1][:, 0:n], op=ALU.mult)
                P.do("pool", "tensor_tensor", out=macc[:, 0:n], in0=macc[:, 0:n], in1=mtmp[:, 0:n], op=ALU.add)
                P.do("dve", "tensor_tensor", out=mtmp[:, 0:n], in0=gs[:, 2, 0:n], in1=pts[2][:, 0:n], op=ALU.mult)
                P.do("pool", "tensor_tensor", out=mT[:, j, 0:n], in0=macc[:, 0:n], in1=mtmp[:, 0:n], op=ALU.add)
            for tt in range(n // 128):
                tok = g0 + tt * 128
                x_ = xt[xi % 2]
                xi += 1
                P.dma("sp", x_, X[b, tok:tok + 128, :], rd=[("X", b, tok // 128)])
                for hf in range(2):
                    pt = ps()
                    for j in range(8):
                        mm(pt, mT[:, j, tt * 128:(tt + 1) * 128], wo[:, j, hf * 512:(hf + 1) * 512], start=(j == 0), stop=(j == 7), r=False)
                    ot = otmp[hf]
                    P.do("dve", "tensor_tensor", out=ot, in0=pt, in1=g1B[slot][:, hf * 512:(hf + 1) * 512], op=ALU.mult)
                    P.do("pool", "tensor_tensor", out=x_[:, hf * 512:(hf + 1) * 512], in0=x_[:, hf * 512:(hf + 1) * 512], in1=ot, op=ALU.add)
                P.dma("sp", X[b, tok:tok + 128, :], x_, wr=[("X", b, tok // 128)])
        P.barrier()

    J = CAP // 128
    NSL = NB * CAP
    NSC = NB * CAPC
    NSLOT = NSL + NSC
    IDXP = sb("IDXP", [128, NB * 16 * J], U32)
    WGTP = sb("WGTP", [128, NB * 16 * J])
    IDXCP = sb("IDXCP", [NSC, 16], U32)
    WGTCP = sb("WGTCP", [NSC, 16])
    IDXPv = IDXP.re("p (s e j) -> p s e j", s=NB, e=16, j=J)
    WGTPv = WGTP.re("p (s e j) -> p s e j", s=NB, e=16, j=J)
    Xflat = X.rearrange("b t d -> (b t) d")

    def phase_moe(l):
        A.reset()
        AFF = A.tile([64, S])
        AFFC = A.tile([64, CTX])
        MX = A.tile([64, CAP])
        IDX = A.tile([64, CAP], U32)
        MXC = A.tile([64, CAPC])
        IDXC = A.tile([64, CAPC], U32)
        P.do("dve", "memset", ap=AFF, constant=0.0)
        P.do("dve", "memset", ap=AFFC, constant=0.0)
        n2B = A.tile([128, D])
        P.dma("sp", n2B, norm2[l:l + 1, :].to_broadcast([128, D]))
        A2B, S2B = {}, {}
        for slot in range(NS):
            A2B[slot] = A.tile([128, D])
            S2B[slot] = A.tile([128, D])
            bcast_row(A2B[slot], slot, 4)
            bcast_row(S2B[slot], slot, 3)
            P.do("dve", "scalar_tensor_tensor", out=A2B[slot], in0=A2B[slot], scalar=1.0, in1=n2B, op0=ALU.add, op1=ALU.mult)
        wr = A.tile([128, 8, NE])
        P.dma("sp", wr, w_router[l].rearrange("(k p) e -> p k e", p=128))
        affw = [A.tile([128, 64]) for _ in range(NB)]
        for b in range(NB):
            P.do("dve", "memset", ap=affw[b], constant=0.0)
        xt = [A.tile([128, D]) for _ in range(2)]
        junk = A.tile([128, D])
        hT2 = [A.tile([128, 8, 128]) for _ in range(2)]
        sm = [A.tile([128, 4]) for _ in range(2)]
        it = 0
        for i in range(NCH):
            for b in range(NB):
                tok = i * 128
                slot = slot_of(b, tok)
                x_ = xt[it % 2]
                h_ = hT2[it % 2]
                m_ = sm[it % 2]
                it += 1
                c0 = 32 * b
                P.dma("sp", x_, X[b, tok:tok + 128, :], rd=[("X", b, i)])
                rms_tile(x_, junk, m_[:, 0:1], m_[:, 1:2], 1.0 / D)
                P.do("dve", "scalar_tensor_tensor", out=x_, in0=x_, scalar=m_[:, 1:2], in1=A2B[slot], op0=ALU.mult, op1=ALU.mult)
                P.do("pool", "tensor_tensor", out=x_, in0=x_, in1=S2B[slot], op=ALU.add)
                P.dma("sp", H2[b * T + tok:b * T + tok + 128, :], x_, wr=[("H2", b, i)])
                for g in range(2):
                    pt = ps()
                    for k4 in range(4):
                        kx = 4 * g + k4
                        P.do("pe", "transpose", out=pt[:, k4 * 128:(k4 + 1) * 128], in_=x_[:, kx * 128:(kx + 1) * 128], identity=ident)
                    evac(h_[:, 4 * g:4 * g + 4, :], pt.re("p (a c) -> p a c", c=128))
                pl = ps()
                for k in range(8):
                    mm(pl[:, 0:NE], h_[:, k, :], wr[:, k, :], start=(k == 0), stop=(k == 7), r=False)
                P.do("dve", "reduce_max", out=m_[:, 2:3], in_=pl[:, 0:NE], axis=AX.X)
                P.do("dve", "tensor_scalar", out=m_[:, 2:3], in0=m_[:, 2:3], scalar1=-1.0, scalar2=None, op0=ALU.mult)
                P.do("act", "activation", out=affw[b][:, c0:c0 + NE], in_=pl[:, 0:NE], func=AF.Exp, bias=m_[:, 2:3], accum_out=m_[:, 3:4])
                P.do("dve", "reciprocal", out=m_[:, 3:4], in_=m_[:, 3:4])
                P.do("dve", "tensor_scalar", out=affw[b][:, c0:c0 + NE], in0=affw[b][:, c0:c0 + NE], scalar1=m_[:, 3:4], scalar2=None, op0=ALU.mult)
                pT = ps()
                P.do("pe", "transpose", out=pT[0:64, 0:128], in_=affw[b], identity=ident)
                dst = AFFC[c0:c0 + NE, tok:tok + 128] if tok < CTX else AFF[c0:c0 + NE, tok - CTX:tok - CTX + 128]
                P.do("act", "activation", out=dst, in_=pT[c0:c0 + NE, 0:128], func=AF.Copy)
        for (af, mx, ix, cap) in ((AFF, MX, IDX, CAP), (AFFC, MXC, IDXC, CAPC)):
            for r in range(cap // 8):
                P.do("dve", "max", out=mx[:, 8 * r:8 * r + 8], in_=af)
                P.do("dve", "max_index", out=ix[:, 8 * r:8 * r + 8], in_max=mx[:, 8 * r:8 * r + 8], in_values=af)
                P.do("dve", "match_replace", out=af, in_to_replace=mx[:, 8 * r:8 * r + 8], in_values=af, imm_value=-1.0)
        P.dma("sp", idx_d, IDX, wr=[("idx",)])
        P.dma("sp", wgt_d, MX, wr=[("idx",)])
        P.dma("sp", idxc_d, IDXC, wr=[("idx",)])
        P.dma("sp", wgtc_d, MXC, wr=[("idx",)])
        P.barrier()
        for s_ in range(NB):
            P.dma("sp", IDXPv[:, s_], idx_d[32 * s_:32 * s_ + NE, :].rearrange("e (j p) -> p e j", p=128), rd=[("idx",)], allow_slow_non_contiguous=True)
            P.dma("sp", WGTPv[:, s_], wgt_d[32 * s_:32 * s_ + NE, :].rearrange("e (j p) -> p e j", p=128), rd=[("idx",)], allow_slow_non_contiguous=True)
            P.dma("sp", IDXCP[s_ * CAPC:(s_ + 1) * CAPC, :], idxc_d[32 * s_:32 * s_ + NE, :].rearrange("e p -> p e"), rd=[("idx",)], allow_slow_non_contiguous=True)
            P.dma("sp", WGTCP[s_ * CAPC:(s_ + 1) * CAPC, :], wgtc_d[32 * s_:32 * s_ + NE, :].rearrange("e p -> p e"), rd=[("idx",)], allow_slow_non_contiguous=True)
            P.do("dve", "tensor_single_scalar", out=IDXPv[:, s_].cast(I32), in_=IDXPv[:, s_].cast(I32), scalar=s_ * T + CTX, op=ALU.add)
            if s_ > 0:
                P.do("dve", "tensor_single_scalar", out=IDXCP[s_ * CAPC:(s_ + 1) * CAPC, :].cast(I32),
                     in_=IDXCP[s_ * CAPC:(s_ + 1) * CAPC, :].cast(I32), scalar=s_ * T, op=ALU.add)
        P.barrier()
        A.reset()
        xeT = A.tile([128, 8, NSLOT], BF16)
        gT = A.tile([128, 16, NSLOT], BF16)
        wst = [A.tile([128, 8, 512]) for _ in range(2)]
        wbf = [A.tile([128, 8, 512], BF16) for _ in range(6)]
        xg = [A.tile([128, D]) for _ in range(2)]
        NTL = NB * J + 1
        yst = A.tile([128, NTL, D])
        tmpa = [A.tile([128, 512]) for _ in range(2)]
        g2B = {}
        for slot in range(NS):
            g2B[slot] = A.tile([128, D])
            bcast_row(g2B[slot], slot, 5)
        tiles = []
        for s_ in range(NB):
            for j in range(J):
                tiles.append((s_ * CAP + j * 128, 128, s_, (lambda e, s_=s_, j=j: IDXPv[:, s_, e, j:j + 1]), (lambda e, s_=s_, j=j: WGTPv[:, s_, e, j:j + 1])))
        tiles.append((NSL, NSC, NB, (lambda e: IDXCP[:, e:e + 1]), (lambda e: WGTCP[:, e:e + 1])))
        blocks = [(a_, min(a_ + 512, NSLOT)) for a_ in range(0, NSLOT, 512)]
        useq = []
        for e in range(NE):
            for fb in range(4):
                useq.append((e, "g", fb))
                useq.append((e, "u", fb))
            for dh in range(2):
                useq.append((e, "d", dh, 0))
                useq.append((e, "d", dh, 1))

        def uload(i):
            u = useq[i]
            e = u[0]
            if u[1] == "g":
                src = w_eg[l, e, :, u[2] * 512:(u[2] + 1) * 512]
            elif u[1] == "u":
                src = w_eu[l, e, :, u[2] * 512:(u[2] + 1) * 512]
            else:
                src = w_ed[l, e, u[3] * 1024:(u[3] + 1) * 1024, u[2] * 512:(u[2] + 1) * 512]
            P.dma("sp", wst[i % 2], src.rearrange("(k p) c -> p k c", p=128))
            if i % 2:
                P.do("act", "activation", out=wbf[i % 6], in_=wst[i % 2], func=AF.Copy)
            else:
                P.do("dve", "tensor_copy", out=wbf[i % 6], in_=wst[i % 2])

        gi = [0]

        def gather(e):
            for (t0, n, slot, icol, wcol) in tiles:
                x_ = xg[gi[0] % 2]
                gi[0] += 1
                ic = icol(e)

                def g(eng, x_=x_, ic=ic, n=n):
                    return eng.indirect_dma_start(out=x_.ap[0:n], out_offset=None, in_=H2,
                                                  in_offset=bass.IndirectOffsetOnAxis(ap=ic.ap, axis=0))
                P.dma_raw("pool", g, list(ic.res), list(x_.res))
                for g2 in range(2):
                    pt = ps()
                    for k4 in range(4):
                        kx = 4 * g2 + k4
                        P.do("pe", "transpose", out=pt[:, k4 * 128:k4 * 128 + n], in_=x_[0:n, kx * 128:(kx + 1) * 128], identity=ident[0:n, 0:n])
                    evac(xeT[:, 4 * g2:4 * g2 + 4, t0:t0 + n], pt.re("p (a c) -> p a c", c=128)[:, :, 0:n])

        uload(0)
        uload(1)
        gather(0)
        npairs = len(useq) // 2
        for p in range(npairs):
            if p + 1 < npairs:
                uload(2 * p + 2)
                uload(2 * p + 3)
            u = useq[2 * p]
            e = u[0]
            w0 = wbf[(2 * p) % 6]
            w1 = wbf[(2 * p + 1) % 6]
            if u[1] == "g":
                fb = u[2]
                for fc in range(4):
                    f = fb * 4 + fc
                    for (s0, s1) in blocks:
                        n = s1 - s0
                        pa = ps()
                        for k in range(8):
                            mm(pa[:, 0:n], w0[:, k, fc * 128:(fc + 1) * 128], xeT[:, k, s0:s1], start=(k == 0), stop=(k == 7), r=False)
                        pu = ps()
                        for k in range(8):
                            mm(pu[:, 0:n], w1[:, k, fc * 128:(fc + 1) * 128], xeT[:, k, s0:s1], start=(k == 0), stop=(k == 7), r=False)
                        ta = tmpa[(fc + s0 // 512) % 2]
                        P.do("act", "activation", out=ta[:, 0:n], in_=pa[:, 0:n], func=AF.Silu)
                        P.do("dve", "tensor_tensor", out=gT[:, f, s0:s1], in0=ta[:, 0:n], in1=pu[:, 0:n], op=ALU.mult)
                if fb == 3 and e + 1 < NE:
                    gather(e + 1)
            else:
                dh = u[2]
                for ti, (t0, n, slot, icol, wcol) in enumerate(tiles):
                    py = ps()
                    for f in range(16):
                        mm(py[0:n, :], gT[:, f, t0:t0 + n], (w0 if f < 8 else w1)[:, f % 8, :], start=(f == 0), stop=(f == 15), r=False)
                    P.do("dve", "scalar_tensor_tensor", out=yst[0:n, ti, dh * 512:(dh + 1) * 512], in0=py[0:n, :], scalar=wcol(e)[0:n],
                         in1=g2B[slot][0:n, dh * 512:(dh + 1) * 512], op0=ALU.mult, op1=ALU.mult)
                if dh == 1:
                    for ti, (t0, n, slot, icol, wcol) in enumerate(tiles):
                        ic = icol(e)

                        def sc(eng, ic=ic, n=n, ti=ti):
                            return eng.indirect_dma_start(out=Xflat, out_offset=bass.IndirectOffsetOnAxis(ap=ic.ap, axis=0),
                                                          in_=yst.ap[0:n, ti, :], in_offset=None, compute_op=ALU.add)
                        P.dma_raw("pool", sc, list(ic.res) + list(yst.res) + [P.R(("Xs",))], [P.R(("Xs",))])
        P.barrier()

    PH = build_program.phases
    for l in range(DEPTH):
        phase_mod(l)
        for b in range(NB):
            phase_proj(l, b)
            if "conv" in PH:
                phase_conv(l, b)
            if "hgrn" in PH:
                phase_hgrn(l, b)
            if "na" in PH:
                phase_na(l, b)
            if "merge" in PH:
                phase_merge(l, b)
        if "moe" in PH:
            phase_moe(l)
        if "stop1" in PH:
            break

    for b in range(NB):
        for j in range(S // 512):
            P.dma("sp", y_out[b, j * 512:(j + 1) * 512, :], X[b, CTX + j * 512:CTX + (j + 1) * 512, :],
                  rd=[("X", b, CTX // 128 + 4 * j + i) for i in range(4)])
    P.barrier()
    P.emit()
    return nc, P


build_program.phases = ("conv", "hgrn", "na", "merge", "moe")


def make_consts():
    c = np.zeros((7, 128, 128), np.float32)
    c[0] = np.eye(128)
    s = np.arange(128)[:, None]
    t = np.arange(128)[None, :]
    c[1] = (s <= t)
    c[2] = (s >= t)
    c[3][:64, :64] = 1.0
    c[3][64:, 64:] = 1.0
    c[4] = 1.0
    kcol = np.arange(128)[:, None] % 64
    q = np.arange(64)[None, :]
    ws = np.clip(q - 8, 0, 48)
    inw = (kcol >= ws) & (kcol < ws + 16)
    c[5][:, :64] = np.where(inw, 0.0, NEG)
    return c


def expand_rpb(rpb):
    p = np.arange(128)
    wpar = (p // 64)[:, None, None]
    kcol = (p % 64)[:, None, None]
    j = np.arange(15)[None, :, None]
    q = np.arange(64)[None, None, :]
    dr = np.minimum(j + wpar, 14) + 0 * q
    dc = np.clip(kcol - q + 15, 0, 30) + 0 * j
    return np.ascontiguousarray(rpb[:, :, dr, dc])


_CACHE = {}


def run(inputs, NB, S, DEPTH, n_cores, dbg=()):
    key = (NB, S, DEPTH, tuple(dbg), build_program.phases)
    if key not in _CACHE:
        _CACHE[key] = build_program(NB, S, DEPTH, dbg)
    nc, P = _CACHE[key]
    consts = make_consts()
    rpbx = expand_rpb(np.asarray(inputs["na_rpb"], np.float32))
    shared = {k: np.ascontiguousarray(np.asarray(v, np.float32)) for k, v in inputs.items()
              if k not in ("x", "c", "ctx", "na_rpb")}
    shared["rpbx"] = rpbx
    shared["consts"] = consts
    in_maps = []
    for i in range(n_cores):
        m = dict(shared)
        m["x"] = np.ascontiguousarray(inputs["x"][i * NB:(i + 1) * NB])
        m["c"] = np.ascontiguousarray(inputs["c"][i * NB:(i + 1) * NB])
        m["ctx"] = np.ascontiguousarray(inputs["ctx"][i * NB:(i + 1) * NB])
        in_maps.append(m)
    res = run_bass_kernel_spmd(nc, in_maps, core_ids=list(range(n_cores)))
    return res


def kernel(**inputs):
    res = run(inputs, 2, 4096, 4, 8)
    return np.concatenate([r["y"] for r in res.results], axis=0).astype(np.float32)
```

```python
import numpy as np
import ml_dtypes
import concourse.bass as bass
import concourse.mybir as mybir
from concourse.bass_utils import run_bass_kernel_spmd

F32 = mybir.dt.float32
F32R = mybir.dt.float32r
BF16 = mybir.dt.bfloat16
U32 = mybir.dt.uint32
I32 = mybir.dt.int32
AF = mybir.ActivationFunctionType
ALU = mybir.AluOpType
AX = mybir.AxisListType

D = 1024
CTX = 256
NIN = 8704
NE = 16
FE = 2048
EPS = 1e-6
NEG = -1e30


class Res:
    __slots__ = ("w", "r")

    def __init__(self):
        self.w = {}
        self.r = {}


class V:
    __slots__ = ("ap", "res")

    def __init__(self, ap, res):
        self.ap = ap
        self.res = res if isinstance(res, list) else [res]

    def __getitem__(self, idx):
        return V(self.ap[idx], self.res)

    def re(self, pat, **kw):
        return V(self.ap.rearrange(pat, **kw), self.res)

    def bc(self, shape):
        return V(self.ap.to_broadcast(shape), self.res)

    def cast(self, dt):
        return V(self.ap.bitcast(dt), self.res)


OUT_KEYS = ("out", "accum_out", "ap")


class Prog:
    def __init__(self, nc, nds=(("sp", 40), ("pool", 40), ("act", 8))):
        self.nc = nc
        self.engs = {"pe": nc.tensor, "act": nc.scalar, "dve": nc.vector, "pool": nc.gpsimd, "sp": nc.sync}
        self.items = {e: [] for e in self.engs}
        self.semh = {}
        self.cnt = {}
        for e in self.engs:
            self.semh[e] = nc.alloc_semaphore("s_" + e)
            self.cnt[e] = 0
        self.dq = {}
        for q, n in nds:
            lst = []
            for i in range(n):
                k = "d_%s%d" % (q, i)
                self.semh[k] = nc.alloc_semaphore(k)
                self.cnt[k] = 0
                lst.append(k)
            self.dq[q] = [lst, 0]
        self.waited = {e: {} for e in self.engs}
        self.rk = {}
        self.nops = 0
        self.keep_names = getattr(Prog, 'keep_names_default', False)
        self.pe_names = []

    def R(self, key):
        r = self.rk.get(key)
        if r is None:
            r = self.rk[key] = Res()
        return r

    def _deps(self, eng, reads, writes):
        need = {}
        for r in reads:
            for s, v in r.w.items():
                if need.get(s, 0) < v:
                    need[s] = v
        for w in writes:
            for s, v in w.w.items():
                if need.get(s, 0) < v:
                    need[s] = v
            for s, v in w.r.items():
                if need.get(s, 0) < v:
                    need[s] = v
        waits = []
        wd = self.waited[eng]
        for s, v in need.items():
            if eng == "pe" and s == "pe":
                continue
            if wd.get(s, 0) >= v:
                continue
            wd[s] = v
            waits.append((s, v))
        return waits

    def _mark(self, tok, reads, writes):
        s, v = tok
        for r in reads:
            r.r[s] = v
        for w in writes:
            w.w = {s: v}
            w.r = {}

    def op(self, eng, fn, reads, writes):
        waits = self._deps(eng, reads, writes)
        self.cnt[eng] += 1
        self._mark((eng, self.cnt[eng]), reads, writes)
        self.items[eng].append((waits, fn, (eng, 1)))
        self.nops += 1

    def dma_raw(self, q, fn, reads, writes):
        lst, i = self.dq[q]
        k = lst[i % len(lst)]
        self.dq[q][1] = i + 1
        waits = self._deps(q, reads, writes)
        pv = self.cnt[k]
        if pv > 0 and self.waited[q].get(k, 0) < pv:
            waits.append((k, pv))
            self.waited[q][k] = pv
        self.cnt[k] += 16
        self._mark((k, self.cnt[k]), reads, writes)
        self.items[q].append((waits, fn, (k, 16)))
        self.nops += 1

    def do(self, eng, meth, **kw):
        reads, writes, args = [], [], {}
        for k, v in kw.items():
            if isinstance(v, V):
                args[k] = v.ap
                if k in OUT_KEYS:
                    writes.extend(v.res)
                else:
                    reads.extend(v.res)
            else:
                args[k] = v

        def fn(e, meth=meth, args=args):
            return getattr(e, meth)(**args)

        self.op(eng, fn, reads, writes)

    def dma(self, q, out, in_, rd=(), wr=(), **kw):
        reads = [self.R(k) for k in rd]
        writes = [self.R(k) for k in wr]
        if isinstance(out, V):
            writes.extend(out.res)
            o = out.ap
        else:
            o = out
        if isinstance(in_, V):
            reads.extend(in_.res)
            i = in_.ap
        else:
            i = in_

        def fn(e, o=o, i=i, kw=kw):
            return e.dma_start(out=o, in_=i, **kw)

        self.dma_raw(q, fn, reads, writes)

    def mark(self, label):
        if not hasattr(self, "marks"):
            self.marks = []
        self.marks.append((label, sum(1 for it in self.items["pe"] if it[1] is not None)))

    def barrier(self):
        for e in self.engs:
            waits = []
            wd = self.waited[e]
            for s, v in self.cnt.items():
                if v > 0 and wd.get(s, 0) < v:
                    wd[s] = v
                    waits.append((s, v))
            if waits:
                self.items[e].append((waits, None, None))

    def emit(self):
        nc = self.nc
        with nc.Block() as block:
            def mk(e):
                def body(eng):
                    for waits, fn, inc in self.items[e]:
                        for (k, v) in waits:
                            eng.wait_ge(self.semh[k], v)
                        if fn is not None:
                            ins = fn(eng)
                            ins.then_inc(self.semh[inc[0]], inc[1])
                            if e == "pe" and self.keep_names:
                                self.pe_names.append(getattr(getattr(ins, "ins", ins), "name", None))
                return body

            block.tensor(mk("pe"))
            block.scalar(mk("act"))
            block.vector(mk("dve"))
            block.gpsimd(mk("pool"))
            block.sync(mk("sp"))


class Arena:
    def __init__(self, nc, nelem):
        self.t = nc.alloc_sbuf_tensor("arena", [128, nelem], F32)
        self.n = nelem
        self.off = 0

    def reset(self):
        self.off = 0

    def tile(self, shape, dt=F32):
        n = 1
        for s in shape[1:]:
            n *= s
        n32 = n if dt in (F32, U32, I32) else (n + 1) // 2
        n32 = (n32 + 1) // 2 * 2
        assert self.off + n32 <= self.n, ("arena overflow", self.off, n32, self.n)
        ap = self.t[:, self.off:self.off + n32]
        self.off += n32
        if dt != F32:
            ap = ap.bitcast(dt)
            ap = ap[:, 0:n]
        if len(shape) == 3:
            ap = ap.rearrange("p (a b) -> p a b", b=shape[2])
        elif len(shape) == 4:
            ap = ap.rearrange("p (a b c) -> p a b c", b=shape[2], c=shape[3])
        if shape[0] != 128:
            ap = ap[0:shape[0]]
        return V(ap, Res())


def make_groups(S):
    groups = [(0, CTX)]
    for t in range(CTX, CTX + S, 512):
        groups.append((t, min(t + 512, CTX + S)))
    return groups


def build_program(NB, S, DEPTH, dbg=()):
    T = CTX + S
    NS = NB + 1
    NCH = T // 128
    ROWS = S // 64
    CAP = 2 * S // NE
    CAPC = 2 * CTX // NE
    nc = bass.Bass("TRN2", target_bir_lowering=False)
    P = Prog(nc)

    def din(name, shape, dt=F32):
        return nc.dram_tensor(name, list(shape), dt, kind="ExternalInput").ap()

    def dscr(name, shape, dt=F32):
        kind = "ExternalOutput" if name in dbg else "Internal"
        return nc.dram_tensor(name, list(shape), dt, kind=kind).ap()

    x_in = din("x", [NB, S, D])
    c_in = din("c", [NB, D])
    ctx_in = din("ctx", [NB, CTX, D])
    cctx_in = din("c_ctx", [D])
    w_mod = din("w_mod", [DEPTH, D, 6 * D])
    b_mod = din("b_mod", [DEPTH, 6 * D])
    norm1 = din("norm1", [DEPTH, D])
    w_in = din("w_in", [DEPTH, D, NIN])
    conv_w = din("conv_w", [DEPTH, 3, 512])
    lb_log = din("hg_lb_logits", [DEPTH, 2, 512])
    hg_norm = din("hg_norm", [DEPTH, 128])
    q_norm = din("na_q_norm", [DEPTH, 64])
    k_norm = din("na_k_norm", [DEPTH, 64])
    rpbx = din("rpbx", [DEPTH, 8, 128, 15, 64])
    w_br = [din("w_br_a", [DEPTH, 512, D]), din("w_br_b", [DEPTH, 512, D]), din("w_br_c", [DEPTH, 512, D])]
    w_out = din("w_out", [DEPTH, D, D])
    norm2 = din("norm2", [DEPTH, D])
    w_router = din("w_router", [DEPTH, D, NE])
    w_eg = din("w_e_gate", [DEPTH, NE, D, FE])
    w_eu = din("w_e_up", [DEPTH, NE, D, FE])
    w_ed = din("w_e_down", [DEPTH, NE, FE, D])
    cst_in = din("consts", [7, 128, 128])
    y_out = nc.dram_tensor("y", [NB, S, D], F32, kind="ExternalOutput").ap()

    X = dscr("X", [NB, T, D])
    H2 = dscr("H2", [NB * T, D])
    PF = [dscr("PF%d" % b_, [NIN, T]) for b_ in range(NB)]
    PK = dscr("PK", [NB, 2, T, 512])
    Y = dscr("Y", [NB, 3, 512, T])
    modrow_d = dscr("modrow", [NS, 6 * D])
    idx_d = dscr("idx_d", [64, CAP], U32)
    wgt_d = dscr("wgt_d", [64, CAP])
    idxc_d = dscr("idxc_d", [64, CAPC], U32)
    wgtc_d = dscr("wgtc_d", [64, CAPC])

    DBG = dscr("DBG", [12, 128, T + 2]) if "DBG" in dbg else None

    def dump(i, v, n):
        if DBG is not None:
            P.dma("sp", DBG[i, :, 0:n], v)

    def sb(name, shape, dt=F32):
        t = nc.alloc_sbuf_tensor(name, list(shape), dt)
        return V(t.ap(), Res())

    cst = sb("cst", [128, 7, 128])
    ident = cst[:, 0, :]
    triU = cst[:, 1, :]
    triL = cst[:, 2, :]
    blkones = cst[:, 3, :]
    ones = cst[:, 4, :]
    maskadd = cst[:, 5, 0:64]
    onesb = sb("onesb", [128, 64], BF16)
    identb = sb("identb", [128, 128], BF16)
    condT = sb("condT", [128, 8, NS])
    modT = sb("modT", [128, 48, NS])
    A1T = sb("A1T", [128, 8, NS])
    n1T = sb("n1T", [128, 8])
    lbT = sb("lbT", [128, DEPTH, 8])
    omlT = sb("omlT", [128, DEPTH, 8])
    nomlT = sb("nomlT", [128, DEPTH, 8])
    lbtmp = sb("lbtmp", [128, 8])
    cwT = sb("cwT", [128, 4, 3])
    hgw = sb("hgw", [128, 1])
    qkw = sb("qkw", [128, 2])
    SMALL = sb("small", [128, 64])

    psum = [V(nc.alloc_psum_tensor("ps%d" % i, [128, 512], F32).ap(), Res()) for i in range(8)]
    psi = [0]

    def ps():
        p = psum[psi[0] % 8]
        psi[0] += 1
        return p

    remaining = nc.sbuf_bytes_remaining
    print("sbuf remaining", remaining)
    arena_elems = (remaining - 2048) // 4
    A = Arena(nc, arena_elems)

    def mm(out, lhsT, rhs, start=True, stop=True, r=True):
        if r:
            lhsT = lhsT.cast(F32R)
            rhs = rhs.cast(F32R)
        P.do("pe", "matmul", out=out, lhsT=lhsT, rhs=rhs, start=start, stop=stop)

    evi = [0]

    def evac(out, in_):
        evi[0] += 1
        if evi[0] % 2:
            P.do("act", "activation", out=out, in_=in_, func=AF.Copy)
        else:
            P.do("dve", "tensor_copy", out=out, in_=in_)

    groups = make_groups(S)
    SBMAX = 2304
    sblocks = []
    cur = []
    for g in groups:
        if cur and (g[1] - cur[0][0]) > SBMAX:
            sblocks.append(cur)
            cur = []
        cur.append(g)
    if cur:
        sblocks.append(cur)

    def pfkeys(b, row0, nrows):
        ks = []
        for rr in range(row0 // 128, (row0 + nrows) // 128):
            for gi in range(len(groups)):
                ks.append(("PF", b, rr, gi))
        return ks

    P.dma("sp", cst, cst_in.rearrange("c p f -> p c f"))
    P.do("dve", "memset", ap=onesb, constant=1.0)
    P.do("dve", "tensor_copy", out=identb, in_=ident)
    for s in range(NB):
        P.dma("sp", condT[:, :, s], c_in[s].rearrange("(k p) -> p k", p=128), allow_slow_non_contiguous=True)
    P.dma("sp", condT[:, :, NB], cctx_in.rearrange("(k p) -> p k", p=128), allow_slow_non_contiguous=True)
    P.do("act", "activation", out=condT, in_=condT, func=AF.Silu)
    for l_ in range(DEPTH):
        for d_ in range(2):
            P.dma("sp", lbT[:, l_, d_ * 4:(d_ + 1) * 4], lb_log[l_, d_].rearrange("(h p) -> p h", p=128), allow_slow_non_contiguous=True)
    P.do("act", "activation", out=lbT, in_=lbT, func=AF.Exp)
    P.do("dve", "tensor_copy", out=lbtmp, in_=lbT[:, 0, :])
    for l in range(1, DEPTH):
        P.do("dve", "tensor_tensor", out=lbtmp, in0=lbtmp, in1=lbT[:, l, :], op=ALU.add)
    P.do("dve", "reciprocal", out=lbtmp, in_=lbtmp)
    for l in range(DEPTH):
        P.do("dve", "tensor_tensor", out=lbT[:, l, :], in0=lbT[:, l, :], in1=lbtmp, op=ALU.mult)
    P.do("dve", "memset", ap=lbT[:, 0, :], constant=0.0)
    for l in range(2, DEPTH):
        P.do("dve", "tensor_tensor", out=lbT[:, l, :], in0=lbT[:, l, :], in1=lbT[:, l - 1, :], op=ALU.add)
    P.do("dve", "tensor_scalar", out=omlT, in0=lbT, scalar1=-1.0, scalar2=1.0, op0=ALU.mult, op1=ALU.add)
    P.do("dve", "tensor_scalar", out=nomlT, in0=omlT, scalar1=-1.0, scalar2=None, op0=ALU.mult)
    for b in range(NB):
        P.dma("sp", X[b, 0:CTX, :], ctx_in[b], wr=[("X", b, j) for j in range(CTX // 128)])
        for j in range(S // 512):
            P.dma("sp", X[b, CTX + j * 512:CTX + (j + 1) * 512, :], x_in[b, j * 512:(j + 1) * 512, :],
                  wr=[("X", b, CTX // 128 + 4 * j + i) for i in range(4)])

    def slot_of(b, tok):
        return NB if tok < CTX else b

    def phase_mod(l):
        A.reset()
        wm = [A.tile([128, 8, 512]) for _ in range(2)]
        bmod_sb = A.tile([1, 6 * D])
        modrow_sb = A.tile([NS, 6 * D])
        P.dma("sp", bmod_sb, b_mod[l:l + 1, :])
        for cg in range(12):
            w = wm[cg % 2]
            P.dma("sp", w, w_mod[l, :, cg * 512:(cg + 1) * 512].rearrange("(k p) c -> p k c", p=128))
            pt = ps()
            for k in range(8):
                mm(pt[0:NS, :], condT[:, k, :], w[:, k, :], start=(k == 0), stop=False, r=False)
            mm(pt[0:NS, :], ones[0:1, 0:NS], bmod_sb[0:1, cg * 512:(cg + 1) * 512], start=False, stop=True, r=False)
            evac(modrow_sb[0:NS, cg * 512:(cg + 1) * 512], pt[0:NS, :])
        P.dma("sp", modrow_d, modrow_sb, wr=[("modrow",)])
        for s_ in range(NS):
            P.dma("sp", modT[:, :, s_], modrow_d[s_].rearrange("(j p) -> p j", p=128), rd=[("modrow",)], allow_slow_non_contiguous=True)
        P.dma("sp", n1T, norm1[l].rearrange("(k p) -> p k", p=128), allow_slow_non_contiguous=True)
        P.do("dve", "tensor_scalar", out=A1T, in0=modT[:, 8:16, :], scalar1=1.0, scalar2=None, op0=ALU.add)
        P.do("dve", "tensor_tensor", out=A1T, in0=A1T, in1=n1T.re("p (k o) -> p k o", o=1).bc([128, 8, NS]), op=ALU.mult)
        for k_ in range(3):
            P.dma("sp", cwT[:, :, k_], conv_w[l, k_].rearrange("(j p) -> p j", p=128), allow_slow_non_contiguous=True)
        P.dma("sp", hgw, hg_norm[l].rearrange("(p o) -> p o", o=1), allow_slow_non_contiguous=True)
        for hh in range(2):
            P.dma("sp", qkw[hh * 64:(hh + 1) * 64, 0:1], q_norm[l].rearrange("(p o) -> p o", o=1), allow_slow_non_contiguous=True)
            P.dma("sp", qkw[hh * 64:(hh + 1) * 64, 1:2], k_norm[l].rearrange("(p o) -> p o", o=1), allow_slow_non_contiguous=True)
        P.do("dve", "tensor_scalar", out=qkw[:, 0:1], in0=qkw[:, 0:1], scalar1=0.125, scalar2=None, op0=ALU.mult)
        P.barrier()

    def rms_tile(xt, junk, ss, rstd, inv_n):
        P.do("act", "activation", out=junk, in_=xt, func=AF.Square, accum_out=ss)
        P.do("dve", "tensor_scalar", out=rstd, in0=ss, scalar1=inv_n, scalar2=EPS, op0=ALU.mult, op1=ALU.add)
        P.do("act", "activation", out=rstd, in_=rstd, func=AF.Sqrt)
        P.do("dve", "reciprocal", out=rstd, in_=rstd)

    def phase_proj(l, b):
        A.reset()
        hT = A.tile([128, 8, SBMAX], BF16)
        wst = [A.tile([128, 8, 512]) for _ in range(2)]
        wb = [A.tile([128, 8, 512], BF16) for _ in range(2)]
        xt = [A.tile([128, 1024]) for _ in range(4)]
        junk = A.tile([128, 1024])
        st = [A.tile([128, 512]) for _ in range(4)]
        sm = [A.tile([128, 2]) for _ in range(4)]
        sti = [0]
        seq = [(si, cg) for si in range(len(sblocks)) for cg in range(17)]

        def wload(i):
            si, cg = seq[i]
            P.dma("sp", wst[i % 2], w_in[l, :, cg * 512:(cg + 1) * 512].rearrange("(k p) c -> p k c", p=128))
            if i % 2:
                P.do("act", "activation", out=wb[i % 2], in_=wst[i % 2], func=AF.Copy)
            else:
                P.do("dve", "tensor_copy", out=wb[i % 2], in_=wst[i % 2])

        def nmt(sbl):
            t0 = sbl[0][0]
            for (g0, g1) in sbl:
                nt = (g1 - g0) // 128
                slot = slot_of(b, g0)
                for tt in range(nt):
                    tok = g0 + tt * 128
                    P.dma("sp", xt[tt], X[b, tok:tok + 128, :], rd=[("X", b, tok // 128)])
                    rms_tile(xt[tt], junk, sm[tt][:, 0:1], sm[tt][:, 1:2], 1.0 / D)
                    P.do("act", "activation", out=xt[tt], in_=xt[tt], func=AF.Copy, scale=sm[tt][:, 1:2])
                for k in range(8):
                    pt = ps()
                    for tt in range(nt):
                        P.do("pe", "transpose", out=pt[:, tt * 128:(tt + 1) * 128], in_=xt[tt][:, k * 128:(k + 1) * 128], identity=ident)
                    P.do("act", "activation", out=hT[:, k, g0 - t0:g1 - t0], in_=pt[:, 0:nt * 128], func=AF.Identity,
                         scale=A1T[:, k, slot:slot + 1], bias=modT[:, k, slot:slot + 1])

        wload(0)
        for i, (si, cg) in enumerate(seq):
            sbl = sblocks[si]
            t0 = sbl[0][0]
            if cg == 0:
                nmt(sbl)
            if i + 1 < len(seq):
                wload(i + 1)
            w = wb[i % 2]
            if cg in (4, 10):
                which = 0 if cg == 4 else 1
                for (g0, g1) in sbl:
                    for tok in range(g0, g1, 128):
                        pt = ps()
                        for k in range(8):
                            mm(pt, hT[:, k, tok - t0:tok - t0 + 128], w[:, k, :], start=(k == 0), stop=(k == 7), r=False)
                        s_ = st[sti[0] % 4]
                        sti[0] += 1
                        evac(s_, pt)
                        P.dma("sp", PK[b, which, tok:tok + 128, :], s_, wr=[("PK", b, which, tok // 128)])
            else:
                for cc in range(4):
                    for gi, (g0, g1) in enumerate(groups):
                        if (g0, g1) not in sbl:
                            continue
                        n = g1 - g0
                        pt = ps()
                        for k in range(8):
                            mm(pt[:, 0:n], w[:, k, cc * 128:(cc + 1) * 128], hT[:, k, g0 - t0:g1 - t0], start=(k == 0), stop=(k == 7), r=False)
                        s_ = st[sti[0] % 4]
                        sti[0] += 1
                        evac(s_[:, 0:n], pt[:, 0:n])
                        r0 = cg * 512 + cc * 128
                        P.dma("sp", PF[b][r0:r0 + 128, g0:g1], s_[:, 0:n], wr=[("PF", b, r0 // 128, gi)])
        P.barrier()

    def phase_conv(l, b):
        A.reset()
        tb = [[A.tile([128, T]) for _ in range(3)] for _ in range(2)]
        yv = [A.tile([128, T]) for _ in range(2)]
        segs = [(0, CTX), (CTX, T)]
        for j in range(4):
            Bt, Ct, Ut = tb[j % 2]
            y = yv[j % 2]
            for i, t_ in enumerate((Bt, Ct, Ut)):
                r0 = i * 512 + j * 128
                P.dma("sp", t_, PF[b][r0:r0 + 128, :], rd=pfkeys(b, r0, 128))
            P.do("dve", "tensor_tensor", out=Ct, in0=Ct, in1=Ut, op=ALU.mult)
            P.do("dve", "tensor_scalar", out=y, in0=Ct, scalar1=cwT[:, j, 1:2], scalar2=None, op0=ALU.mult)
            for (a, e) in segs:
                P.do("dve", "scalar_tensor_tensor", out=y[:, a + 1:e], in0=Ct[:, a:e - 1], scalar=cwT[:, j, 0:1],
                     in1=y[:, a + 1:e], op0=ALU.mult, op1=ALU.add)
                P.do("dve", "scalar_tensor_tensor", out=y[:, a:e - 1], in0=Ct[:, a + 1:e], scalar=cwT[:, j, 2:3],
                     in1=y[:, a:e - 1], op0=ALU.mult, op1=ALU.add)
            P.do("dve", "tensor_tensor", out=y, in0=y, in1=Bt, op=ALU.mult)
            P.dma("pool", Y[b, 0, j * 128:(j + 1) * 128, :], y, wr=[("Y", b, 0, j)])
        P.barrier()

    DKS = 128 ** -0.5

    def phase_hgrn(l, b):
        A.reset()
        HC = 64
        NC = T // HC
        q = A.tile([128, T])
        kk = A.tile([128, T])
        b2 = A.tile([128, T])
        G = A.tile([128, T + 2])
        qt = A.tile([128, T])
        qh = A.tile([128, T])
        kb = A.tile([128, T])
        Vt = A.tile([HC, NC, 128])
        O = A.tile([128, T])
        Sring = [A.tile([128, 128]) for _ in range(4)]
        AmD = [[A.tile([HC, HC]) for _ in range(3)] for _ in range(2)]
        qb = A.tile([128, NC, 32])
        kb2 = A.tile([128, NC, 32])
        for d_ in range(2):
            for a_ in AmD[d_]:
                P.do("dve", "memset", ap=a_, constant=0.0)
        kbT = [A.tile([HC, 128]) for _ in range(3)]
        eend = A.tile([128, NC])
        P.do("dve", "memset", ap=G[:, 0:1], constant=0.0)

        def c3(v):
            return v.re("p (c t) -> p c t", t=HC)

        def c32(v):
            return v.re("p (c t) -> p c t", t=32)

        for h in range(4):
            P.dma("sp", q, PF[b][1536 + h * 128:1536 + (h + 1) * 128, :], rd=pfkeys(b, 1536 + h * 128, 128))
            P.do("act", "activation", out=q, in_=q, func=AF.Silu)
            P.dma("sp", Vt, PK[b, 0, :, h * 128:(h + 1) * 128].rearrange("(c p) d -> p c d", p=HC),
                  rd=[("PK", b, 0, i) for i in range(NCH)])
            for d in range(2):
                ix = d * 4 + h
                r0 = 2560 + d * 512 + h * 128
                P.dma("sp", kk, PF[b][r0:r0 + 128, :], rd=pfkeys(b, r0, 128))
                P.do("act", "activation", out=kk, in_=kk, func=AF.Sigmoid)
                P.do("dve", "tensor_scalar", out=b2, in0=kk, scalar1=omlT[:, l, ix:ix + 1], scalar2=lbT[:, l, ix:ix + 1],
                     op0=ALU.mult, op1=ALU.add)
                P.do("act", "activation", out=b2, in_=b2, func=AF.Ln)
                P.do("dve", "tensor_scalar", out=kk, in0=kk, scalar1=nomlT[:, l, ix:ix + 1], scalar2=omlT[:, l, ix:ix + 1],
                     op0=ALU.mult, op1=ALU.add)
                P.do("dve", "tensor_tensor_scan", out=G[:, 1:T + 1], data0=ones[:, 0:1].bc([128, T]), data1=b2, initial=0.0,
                     op0=ALU.mult, op1=ALU.add)
                Gi = c3(G[:, 1:T + 1])
                Hx = c3(G[:, 0:T])
                A3 = Gi if d == 0 else Hx
                Gs = Hx[:, :, 0:1]
                Ge = Gi[:, :, HC - 1:HC]
                A32 = c32(G[:, 1:T + 1]) if d == 0 else c32(G[:, 0:T])
                mid = A32[:, :, 15:16]
                sg = 1.0 if d == 0 else -1.0
                P.do("dve", "tensor_tensor", out=eend.re("p (c o) -> p c o", o=1), in0=Ge, in1=Gs, op=ALU.subtract)
                P.do("act", "activation", out=eend, in_=eend, func=AF.Exp)
                P.do("dve", "tensor_tensor", out=c32(b2), in0=A32, in1=mid.bc([128, T // 32, 32]), op=ALU.subtract)
                P.do("act", "activation", out=qt, in_=b2, func=AF.Exp, scale=sg)
                P.do("dve", "scalar_tensor_tensor", out=qt, in0=q, scalar=DKS, in1=qt, op0=ALU.mult, op1=ALU.mult)
                P.do("act", "activation", out=b2, in_=b2, func=AF.Exp, scale=-sg)
                P.do("dve", "tensor_tensor", out=b2, in0=b2, in1=kk, op=ALU.mult)
                if d == 0:
                    P.do("dve", "tensor_tensor", out=c3(qh), in0=Gi, in1=Gs.bc([128, NC, HC]), op=ALU.subtract)
                    P.do("dve", "tensor_tensor", out=c3(kb), in0=Ge.bc([128, NC, HC]), in1=Gi, op=ALU.subtract)
                else:
                    P.do("dve", "tensor_tensor", out=c3(qh), in0=Ge.bc([128, NC, HC]), in1=Hx, op=ALU.subtract)
                    P.do("dve", "tensor_tensor", out=c3(kb), in0=Hx, in1=Gs.bc([128, NC, HC]), op=ALU.subtract)
                P.do("act", "activation", out=qh, in_=qh, func=AF.Exp)
                P.do("dve", "scalar_tensor_tensor", out=qh, in0=q, scalar=DKS, in1=qh, op0=ALU.mult, op1=ALU.mult)
                P.do("act", "activation", out=kb, in_=kb, func=AF.Exp)
                P.do("dve", "tensor_tensor", out=kb, in0=kb, in1=kk, op=ALU.mult)
                if h == 0 and d == 0:
                    dump(0, q, T); dump(1, kk, T); dump(2, G, T + 2); dump(3, qt, T); dump(4, b2, T); dump(5, qh, T); dump(6, kb, T)
                    dump(7, eend, NC)
                q3 = c3(q)
                k3 = c3(kk)
                if d == 0:
                    Bd = Gi[:, :, 31:32].bc([128, NC, 32])
                    P.do("dve", "tensor_tensor", out=qb, in0=Gi[:, :, 32:64], in1=Bd, op=ALU.subtract)
                    P.do("dve", "tensor_tensor", out=kb2, in0=Bd, in1=Gi[:, :, 0:32], op=ALU.subtract)
                    qsl, ksl = q3[:, :, 32:64], k3[:, :, 0:32]
                else:
                    Bd = Hx[:, :, 32:33].bc([128, NC, 32])
                    P.do("dve", "tensor_tensor", out=qb, in0=Bd, in1=Hx[:, :, 0:32], op=ALU.subtract)
                    P.do("dve", "tensor_tensor", out=kb2, in0=Hx[:, :, 32:64], in1=Bd, op=ALU.subtract)
                    qsl, ksl = q3[:, :, 0:32], k3[:, :, 32:64]
                P.do("act", "activation", out=qb, in_=qb, func=AF.Exp)
                P.do("dve", "scalar_tensor_tensor", out=qb, in0=qsl, scalar=DKS, in1=qb, op0=ALU.mult, op1=ALU.mult)
                P.do("act", "activation", out=kb2, in_=kb2, func=AF.Exp)
                P.do("dve", "tensor_tensor", out=kb2, in0=kb2, in1=ksl, op=ALU.mult)
                P.do("dve", "memset", ap=Sring[0], constant=0.0)
                ncx = CTX // HC
                order = list(range(NC)) if d == 0 else list(range(ncx - 1, -1, -1)) + list(range(NC - 1, ncx - 1, -1))
                tri = triU if d == 0 else triL
                def stA(ci):
                    c = order[ci]
                    c0 = c * HC
                    cs = slice(c0, c0 + HC)
                    pa = ps()
                    mm(pa[0:32, 0:32], b2[:, c0:c0 + 32], qt[:, c0:c0 + 32], r=False)
                    mm(pa[32:64, 32:64], b2[:, c0 + 32:c0 + 64], qt[:, c0 + 32:c0 + 64], r=False)
                    ob = (slice(0, 32), slice(32, 64)) if d == 0 else (slice(32, 64), slice(0, 32))
                    mm(pa[ob[0], ob[1]], kb2[:, c, :], qb[:, c, :], r=False)
                    P.do("pe", "transpose", out=pa[0:HC, 128:256], in_=kb[:, cs], identity=ident)
                    am = AmD[d][ci % 3]
                    P.do("dve", "tensor_tensor", out=am[0:32, 0:32], in0=pa[0:32, 0:32], in1=tri[0:32, 0:32], op=ALU.mult)
                    P.do("dve", "tensor_tensor", out=am[32:64, 32:64], in0=pa[32:64, 32:64], in1=tri[32:64, 32:64], op=ALU.mult)
                    P.do("act", "activation", out=am[ob[0], ob[1]], in_=pa[ob[0], ob[1]], func=AF.Copy)
                    P.do("act", "activation", out=kbT[ci % 3], in_=pa[0:HC, 128:256], func=AF.Copy)

                def stB(ci):
                    c = order[ci]
                    pS = ps()
                    mm(pS[:, 0:128], kbT[ci % 3], Vt[:, c, :], r=False)
                    P.do("dve", "scalar_tensor_tensor", out=Sring[(ci + 1) % 4], in0=Sring[ci % 4], scalar=eend[:, c:c + 1],
                         in1=pS[:, 0:128], op0=ALU.mult, op1=ALU.add)

                def stC(ci):
                    c = order[ci]
                    cs = slice(c * HC, (c + 1) * HC)
                    po = ps()
                    mm(po[:, 0:HC], Sring[ci % 4], qh[:, cs], start=True, stop=False, r=False)
                    mm(po[:, 0:HC], Vt[:, c, :], AmD[d][ci % 3], start=False, stop=True, r=False)
                    if d == 0:
                        P.do("act", "activation", out=O[:, cs], in_=po[:, 0:HC], func=AF.Copy)
                    else:
                        P.do("dve", "tensor_tensor", out=O[:, cs], in0=O[:, cs], in1=po[:, 0:HC], op=ALU.add)

                nord = len(order)
                stA(0)
                if nord > 1:
                    stA(1)
                stB(0)
                for ci in range(nord):
                    if ci + 2 < nord:
                        stA(ci + 2)
                    if ci + 1 < nord:
                        stB(ci + 1)
                    stC(ci)
            if h == 0:
                dump(8, O, T)
            P.do("act", "activation", out=b2, in_=O, func=AF.Square)
            for (g0, g1) in groups:
                pt = ps()
                mm(pt[:, 0:g1 - g0], ones, b2[:, g0:g1], r=False)
                P.do("dve", "tensor_scalar", out=qt[:, g0:g1], in0=pt[:, 0:g1 - g0], scalar1=1.0 / 128, scalar2=EPS,
                     op0=ALU.mult, op1=ALU.add)
            P.do("act", "activation", out=qt, in_=qt, func=AF.Sqrt)
            P.do("dve", "reciprocal", out=qt, in_=qt)
            P.do("dve", "tensor_tensor", out=O, in0=O, in1=qt, op=ALU.mult)
            r0 = 3584 + h * 128
            P.dma("sp", kk, PF[b][r0:r0 + 128, :], rd=pfkeys(b, r0, 128))
            P.do("act", "activation", out=kk, in_=kk, func=AF.Silu)
            P.do("dve", "scalar_tensor_tensor", out=O, in0=O, scalar=hgw[:, 0:1], in1=kk, op0=ALU.mult, op1=ALU.mult)
            P.dma("sp", Y[b, 1, h * 128:(h + 1) * 128, :], O, wr=[("Y", b, 1, h)])
        P.barrier()

    def phase_na(l, b):
        NT = S // 128
        for hp in range(4):
            A.reset()
            Q = A.tile([128, T])
            Kt = A.tile([128, T])
            tmp = A.tile([128, T])
            YC = A.tile([128, T])
            Vst = A.tile([128, NT, 128])
            Ve = A.tile([128, NT, 128], BF16)
            Vo = A.tile([128, NT, 128], BF16)
            Vc = A.tile([128, 2, 128], BF16)
            bias = [A.tile([128, 15, 64]) for _ in range(2)]
            biasb = [A.tile([128, 15, 64], BF16) for _ in range(2)]
            Qb = A.tile([128, T], BF16)
            Kb = A.tile([128, T], BF16)
            PT = [A.tile([128, 512], BF16) for _ in range(3)]
            rden = [A.tile([128, 256]) for _ in range(3)]
            for (tl, tlb, r0, wcol) in ((Q, Qb, 4096, 0), (Kt, Kb, 4608, 1)):
                P.dma("sp", tl, PF[b][r0 + hp * 128:r0 + (hp + 1) * 128, :], rd=pfkeys(b, r0 + hp * 128, 128))
                P.do("act", "activation", out=tmp, in_=tl, func=AF.Square)
                for (g0, g1) in groups:
                    pt = ps()
                    mm(pt[:, 0:g1 - g0], blkones, tmp[:, g0:g1], r=False)
                    P.do("dve", "tensor_scalar", out=YC[:, g0:g1], in0=pt[:, 0:g1 - g0], scalar1=1.0 / 64, scalar2=EPS,
                         op0=ALU.mult, op1=ALU.add)
                P.do("act", "activation", out=YC, in_=YC, func=AF.Sqrt)
                P.do("dve", "reciprocal", out=YC, in_=YC)
                P.do("dve", "scalar_tensor_tensor", out=tlb, in0=tl, scalar=qkw[:, wcol:wcol + 1], in1=YC, op0=ALU.mult, op1=ALU.mult)
            pkk = [("PK", b, 1, i) for i in range(NCH)]
            P.dma("sp", Vst, PK[b, 1, CTX:T, hp * 128:(hp + 1) * 128].rearrange("(i p) d -> p i d", p=128), rd=pkk)
            P.do("dve", "tensor_copy", out=Ve, in_=Vst)
            P.dma("sp", Vst[:, 0:NT - 1, :], PK[b, 1, CTX + 64:T - 64, hp * 128:(hp + 1) * 128].rearrange("(i p) d -> p i d", p=128), rd=pkk)
            P.do("dve", "tensor_copy", out=Vo[:, 0:NT - 1, :], in_=Vst[:, 0:NT - 1, :])
            P.dma("sp", Vst[:, 0:2, :], PK[b, 1, 0:CTX, hp * 128:(hp + 1) * 128].rearrange("(i p) d -> p i d", p=128), rd=pkk)
            P.do("dve", "tensor_copy", out=Vc, in_=Vst[:, 0:2, :])
            for hh in range(2):
                P.dma("sp", bias[hh], rpbx[l, 2 * hp + hh])
                P.do("dve", "tensor_tensor", out=biasb[hh], in0=bias[hh], in1=maskadd.re("p (o q) -> p o q", o=1).bc([128, 15, 64]), op=ALU.add)
            cnt = [0]

            def scores(qc0, nq, chunks, hh):
                hs = slice(hh * 64, (hh + 1) * 64)
                pt = ps()
                for ci, (ktok, bia, vt) in enumerate(chunks):
                    o_ = pt[:, ci * nq:(ci + 1) * nq]
                    mm(o_, Kb[hs, ktok:ktok + 128], Qb[hs, qc0:qc0 + nq], start=True, stop=(bia is None), r=False)
                    if bia is not None:
                        mm(o_, identb, bia, start=False, stop=True, r=False)
                return pt

            def rest(pt, qc0, nq, chunks, hh):
                hs = slice(hh * 64, (hh + 1) * 64)
                nc_ = len(chunks) * nq
                p_ = PT[cnt[0] % 3]
                rd_ = rden[cnt[0] % 3]
                cnt[0] += 1
                P.do("act", "activation", out=p_[:, 0:nc_], in_=pt[:, 0:nc_], func=AF.Exp)
                po = ps()
                for ci, (ktok, bia, vt) in enumerate(chunks):
                    mm(po[hs, 0:nq], vt, p_[:, ci * nq:(ci + 1) * nq], start=(ci == 0), stop=(ci == len(chunks) - 1), r=False)
                for ci, (ktok, bia, vt) in enumerate(chunks):
                    mm(po[hs, nq:2 * nq], onesb, p_[:, ci * nq:(ci + 1) * nq], start=(ci == 0), stop=(ci == len(chunks) - 1), r=False)
                P.do("dve", "reciprocal", out=rd_[hs, 0:nq], in_=po[hs, nq:2 * nq])
                P.do("dve", "tensor_tensor", out=YC[hs, qc0:qc0 + nq], in0=po[hs, 0:nq], in1=rd_[hs, 0:nq], op=ALU.mult)

            units_h = [[], []]
            for hh in range(2):
                units = units_h[hh]
                vs = slice(hh * 64, (hh + 1) * 64)
                cch = [(0, None, Vc[:, 0, vs]), (128, None, Vc[:, 1, vs])]
                units.append((0, CTX, cch, hh))
                for r in range(ROWS):
                    r0 = min(max(r - 4, 0), ROWS - 8)
                    dr0 = r0 - r + 7
                    chunks = []
                    for c in range(4):
                        rho = r0 + 2 * c
                        vt = Ve[:, rho // 2, vs] if rho % 2 == 0 else Vo[:, (rho - 1) // 2, vs]
                        chunks.append((CTX + 64 * rho, biasb[hh][:, dr0 + 2 * c, :], vt))
                    units.append((CTX + 64 * r, 64, chunks + cch, hh))
            units = [u for pair in zip(units_h[0], units_h[1]) for u in pair]
            nxt = scores(*units[0])
            for ui, u in enumerate(units):
                cur = nxt
                if ui + 1 < len(units):
                    nxt = scores(*units[ui + 1])
                rest(cur, *u)
            P.dma("sp", Y[b, 2, hp * 128:(hp + 1) * 128, :], YC, wr=[("Y", b, 2, hp)])
            P.barrier()

    def bcast_row(tile_, slot, j):
        P.dma("sp", tile_, modrow_d[slot:slot + 1, j * D:(j + 1) * D].to_broadcast([128, D]), rd=[("modrow",)])

    def phase_merge(l, b):
        A.reset()
        wbr = [A.tile([128, 4, 1024], BF16) for _ in range(3)]
        wo = A.tile([128, 8, 1024], BF16)
        wst = [A.tile([128, 4, 1024]) for _ in range(2)]
        yst = [A.tile([128, 4, 512]) for _ in range(2)]
        yb = [A.tile([128, 4, 512], BF16) for _ in range(3)]
        mT = A.tile([128, 8, 512], BF16)
        gst = [A.tile([128, 3, 512]) for _ in range(2)]
        macc = A.tile([128, 512])
        mtmp = A.tile([128, 512])
        xt = [A.tile([128, 1024]) for _ in range(2)]
        otmp = [A.tile([128, 512]) for _ in range(2)]
        g1B = {}
        for slot in (b, NB):
            g1B[slot] = A.tile([128, 1024])
            bcast_row(g1B[slot], slot, 2)
        wi = 0
        for i in range(3):
            P.dma("sp", wst[wi % 2], w_br[i][l].rearrange("(k p) c -> p k c", p=128))
            P.do("dve", "tensor_copy", out=wbr[i], in_=wst[wi % 2])
            wi += 1
        for hf in range(2):
            P.dma("sp", wst[wi % 2], w_out[l, hf * 512:(hf + 1) * 512, :].rearrange("(k p) c -> p k c", p=128))
            P.do("act", "activation", out=wo[:, hf * 4:(hf + 1) * 4, :], in_=wst[wi % 2], func=AF.Copy)
            wi += 1
        yi = 0
        gi_ = 0
        xi = 0
        for gi, (g0, g1) in enumerate(groups):
            n = g1 - g0
            slot = slot_of(b, g0)
            for i in range(3):
                ys = yst[yi % 2]
                yi += 1
                P.dma("sp", ys[:, :, 0:n], Y[b, i, :, g0:g1].rearrange("(k p) t -> p k t", p=128), rd=[("Y", b, i, k) for k in range(4)])
                if i == 1:
                    P.do("act", "activation", out=yb[i][:, :, 0:n], in_=ys[:, :, 0:n], func=AF.Copy)
                else:
                    P.do("dve", "tensor_copy", out=yb[i][:, :, 0:n], in_=ys[:, :, 0:n])
            for j in range(8):
                gs = gst[gi_ % 2]
                gi_ += 1
                for i in range(3):
                    r0 = 5632 + i * 1024 + j * 128
                    P.dma("sp", gs[:, i, 0:n], PF[b][r0:r0 + 128, g0:g1], rd=[("PF", b, r0 // 128, gi)])
                P.do("act", "activation", out=gs[:, :, 0:n], in_=gs[:, :, 0:n], func=AF.Sigmoid)
                pts = []
                for i in range(3):
                    pt = ps()
                    for k in range(4):
                        mm(pt[:, 0:n], wbr[i][:, k, j * 128:(j + 1) * 128], yb[i][:, k, 0:n], start=(k == 0), stop=(k == 3), r=False)
                    pts.append(pt)
                P.do("dve", "tensor_tensor", out=macc[:, 0:n], in0=gs[:, 0, 0:n], in1=pts[0][:, 0:n], op=ALU.mult)
                P.do("dve", "tensor_tensor", out=mtmp[:, 0:n], in0=gs[:, 1, 0:n], in1=pts[1][:, 0:n], op=ALU.mult)
                P.do("dve", "tensor_tensor", out=macc[:, 0:n], in0=macc[:, 0:n], in1=mtmp[:, 0:n], op=ALU.add)
                P.do("dve", "tensor_tensor", out=mtmp[:, 0:n], in0=gs[:, 2, 0:n], in1=pts[2][:, 0:n], op=ALU.mult)
                P.do("dve", "tensor_tensor", out=mT[:, j, 0:n], in0=macc[:, 0:n], in1=mtmp[:, 0:n], op=ALU.add)
            for tt in range(n // 128):
                tok = g0 + tt * 128
                x_ = xt[xi % 2]
                xi += 1
                P.dma("sp", x_, X[b, tok:tok + 128, :], rd=[("X", b, tok // 128)])
                for hf in range(2):
                    pt = ps()
                    for j in range(8):
                        mm(pt, mT[:, j, tt * 128:(tt + 1) * 128], wo[:, j, hf * 512:(hf + 1) * 512], start=(j == 0), stop=(j == 7), r=False)
                    ot = otmp[hf]
                    P.do("dve", "tensor_tensor", out=ot, in0=pt, in1=g1B[slot][:, hf * 512:(hf + 1) * 512], op=ALU.mult)
                    P.do("dve", "tensor_tensor", out=x_[:, hf * 512:(hf + 1) * 512], in0=x_[:, hf * 512:(hf + 1) * 512], in1=ot, op=ALU.add)
                P.dma("sp", X[b, tok:tok + 128, :], x_, wr=[("X", b, tok // 128)])
        P.barrier()

    J = CAP // 128
    NSL = NB * CAP
    NSC = NB * CAPC
    NSLOT = NSL + NSC
    IDXP = sb("IDXP", [128, NB * 16 * J], U32)
    WGTP = sb("WGTP", [128, NB * 16 * J])
    IDXCP = sb("IDXCP", [NSC, 16], U32)
    WGTCP = sb("WGTCP", [NSC, 16])
    IDXPv = IDXP.re("p (s e j) -> p s e j", s=NB, e=16, j=J)
    WGTPv = WGTP.re("p (s e j) -> p s e j", s=NB, e=16, j=J)
    Xflat = X.rearrange("b t d -> (b t) d")

    def phase_moe(l):
        A.reset()
        AFF = A.tile([64, S])
        AFFC = A.tile([64, CTX])
        MX = A.tile([64, CAP])
        IDX = A.tile([64, CAP], U32)
        MXC = A.tile([64, CAPC])
        IDXC = A.tile([64, CAPC], U32)
        P.do("dve", "memset", ap=AFF, constant=0.0)
        P.do("dve", "memset", ap=AFFC, constant=0.0)
        n2B = A.tile([128, D])
        P.dma("sp", n2B, norm2[l:l + 1, :].to_broadcast([128, D]))
        A2B, S2B = {}, {}
        for slot in range(NS):
            A2B[slot] = A.tile([128, D])
            S2B[slot] = A.tile([128, D])
            bcast_row(A2B[slot], slot, 4)
            bcast_row(S2B[slot], slot, 3)
            P.do("dve", "scalar_tensor_tensor", out=A2B[slot], in0=A2B[slot], scalar=1.0, in1=n2B, op0=ALU.add, op1=ALU.mult)
        wr = A.tile([128, 8, NE])
        P.dma("sp", wr, w_router[l].rearrange("(k p) e -> p k e", p=128))
        affw = [A.tile([128, 64]) for _ in range(NB)]
        for b in range(NB):
            P.do("dve", "memset", ap=affw[b], constant=0.0)
        xt = [A.tile([128, D]) for _ in range(2)]
        junk = A.tile([128, D])
        hT2 = [A.tile([128, 8, 128]) for _ in range(2)]
        sm = [A.tile([128, 4]) for _ in range(2)]
        it = 0
        for i in range(NCH):
            for b in range(NB):
                tok = i * 128
                slot = slot_of(b, tok)
                x_ = xt[it % 2]
                h_ = hT2[it % 2]
                m_ = sm[it % 2]
                it += 1
                c0 = 32 * b
                P.dma("sp", x_, X[b, tok:tok + 128, :], rd=[("X", b, i)])
                rms_tile(x_, junk, m_[:, 0:1], m_[:, 1:2], 1.0 / D)
                P.do("dve", "scalar_tensor_tensor", out=x_, in0=x_, scalar=m_[:, 1:2], in1=A2B[slot], op0=ALU.mult, op1=ALU.mult)
                P.do("dve", "tensor_tensor", out=x_, in0=x_, in1=S2B[slot], op=ALU.add)
                P.dma("sp", H2[b * T + tok:b * T + tok + 128, :], x_, wr=[("H2", b, i)])
                for g in range(2):
                    pt = ps()
                    for k4 in range(4):
                        kx = 4 * g + k4
                        P.do("pe", "transpose", out=pt[:, k4 * 128:(k4 + 1) * 128], in_=x_[:, kx * 128:(kx + 1) * 128], identity=ident)
                    evac(h_[:, 4 * g:4 * g + 4, :], pt.re("p (a c) -> p a c", c=128))
                pl = ps()
                for k in range(8):
                    mm(pl[:, 0:NE], h_[:, k, :], wr[:, k, :], start=(k == 0), stop=(k == 7), r=False)
                P.do("dve", "reduce_max", out=m_[:, 2:3], in_=pl[:, 0:NE], axis=AX.X)
                P.do("dve", "tensor_scalar", out=m_[:, 2:3], in0=m_[:, 2:3], scalar1=-1.0, scalar2=None, op0=ALU.mult)
                P.do("act", "activation", out=affw[b][:, c0:c0 + NE], in_=pl[:, 0:NE], func=AF.Exp, bias=m_[:, 2:3], accum_out=m_[:, 3:4])
                P.do("dve", "reciprocal", out=m_[:, 3:4], in_=m_[:, 3:4])
                P.do("dve", "tensor_scalar", out=affw[b][:, c0:c0 + NE], in0=affw[b][:, c0:c0 + NE], scalar1=m_[:, 3:4], scalar2=None, op0=ALU.mult)
                pT = ps()
                P.do("pe", "transpose", out=pT[0:64, 0:128], in_=affw[b], identity=ident)
                dst = AFFC[c0:c0 + NE, tok:tok + 128] if tok < CTX else AFF[c0:c0 + NE, tok - CTX:tok - CTX + 128]
                P.do("act", "activation", out=dst, in_=pT[c0:c0 + NE, 0:128], func=AF.Copy)
        for (af, mx, ix, cap) in ((AFF, MX, IDX, CAP), (AFFC, MXC, IDXC, CAPC)):
            for r in range(cap // 8):
                P.do("dve", "max", out=mx[:, 8 * r:8 * r + 8], in_=af)
                P.do("dve", "max_index", out=ix[:, 8 * r:8 * r + 8], in_max=mx[:, 8 * r:8 * r + 8], in_values=af)
                P.do("dve", "match_replace", out=af, in_to_replace=mx[:, 8 * r:8 * r + 8], in_values=af, imm_value=-1.0)
        P.dma("sp", idx_d, IDX, wr=[("idx",)])
        P.dma("sp", wgt_d, MX, wr=[("idx",)])
        P.dma("sp", idxc_d, IDXC, wr=[("idx",)])
        P.dma("sp", wgtc_d, MXC, wr=[("idx",)])
        P.barrier()
        P.mark("moe_ab")
        for s_ in range(NB):
            P.dma("sp", IDXPv[:, s_], idx_d[32 * s_:32 * s_ + NE, :].rearrange("e (j p) -> p e j", p=128), rd=[("idx",)], allow_slow_non_contiguous=True)
            P.dma("sp", WGTPv[:, s_], wgt_d[32 * s_:32 * s_ + NE, :].rearrange("e (j p) -> p e j", p=128), rd=[("idx",)], allow_slow_non_contiguous=True)
            P.dma("sp", IDXCP[s_ * CAPC:(s_ + 1) * CAPC, :], idxc_d[32 * s_:32 * s_ + NE, :].rearrange("e p -> p e"), rd=[("idx",)], allow_slow_non_contiguous=True)
            P.dma("sp", WGTCP[s_ * CAPC:(s_ + 1) * CAPC, :], wgtc_d[32 * s_:32 * s_ + NE, :].rearrange("e p -> p e"), rd=[("idx",)], allow_slow_non_contiguous=True)
            P.do("dve", "tensor_single_scalar", out=IDXPv[:, s_].cast(I32), in_=IDXPv[:, s_].cast(I32), scalar=s_ * T + CTX, op=ALU.add)
            if s_ > 0:
                P.do("dve", "tensor_single_scalar", out=IDXCP[s_ * CAPC:(s_ + 1) * CAPC, :].cast(I32),
                     in_=IDXCP[s_ * CAPC:(s_ + 1) * CAPC, :].cast(I32), scalar=s_ * T, op=ALU.add)
        P.barrier()
        A.reset()
        xeT = A.tile([128, 8, NSLOT], BF16)
        gT = A.tile([128, 16, NSLOT], BF16)
        wst = [A.tile([128, 8, 512]) for _ in range(2)]
        wbf = [A.tile([128, 8, 512], BF16) for _ in range(6)]
        xg = [A.tile([128, D]) for _ in range(2)]
        NTL = NB * J + 1
        yst = A.tile([128, NTL, D])
        tmpa = [A.tile([128, 512]) for _ in range(2)]
        g2B = {}
        for slot in range(NS):
            g2B[slot] = A.tile([128, D])
            bcast_row(g2B[slot], slot, 5)
        tiles = []
        for s_ in range(NB):
            for j in range(J):
                tiles.append((s_ * CAP + j * 128, 128, s_, (lambda e, s_=s_, j=j: IDXPv[:, s_, e, j:j + 1]), (lambda e, s_=s_, j=j: WGTPv[:, s_, e, j:j + 1])))
        tiles.append((NSL, NSC, NB, (lambda e: IDXCP[:, e:e + 1]), (lambda e: WGTCP[:, e:e + 1])))
        blocks = [(a_, min(a_ + 512, NSLOT)) for a_ in range(0, NSLOT, 512)]
        useq = []
        for e in range(NE):
            for fb in range(4):
                useq.append((e, "g", fb))
                useq.append((e, "u", fb))
            for dh in range(2):
                useq.append((e, "d", dh, 0))
                useq.append((e, "d", dh, 1))

        def uload(i):
            u = useq[i]
            e = u[0]
            if u[1] == "g":
                src = w_eg[l, e, :, u[2] * 512:(u[2] + 1) * 512]
            elif u[1] == "u":
                src = w_eu[l, e, :, u[2] * 512:(u[2] + 1) * 512]
            else:
                src = w_ed[l, e, u[3] * 1024:(u[3] + 1) * 1024, u[2] * 512:(u[2] + 1) * 512]
            P.dma("sp", wst[i % 2], src.rearrange("(k p) c -> p k c", p=128))
            if i % 2:
                P.do("act", "activation", out=wbf[i % 6], in_=wst[i % 2], func=AF.Copy)
            else:
                P.do("dve", "tensor_copy", out=wbf[i % 6], in_=wst[i % 2])

        gi = [0]

        def gather(e):
            for (t0, n, slot, icol, wcol) in tiles:
                x_ = xg[gi[0] % 2]
                gi[0] += 1
                ic = icol(e)

                def g(eng, x_=x_, ic=ic, n=n):
                    return eng.indirect_dma_start(out=x_.ap[0:n], out_offset=None, in_=H2,
                                                  in_offset=bass.IndirectOffsetOnAxis(ap=ic.ap, axis=0))
                P.dma_raw("pool", g, list(ic.res), list(x_.res))
                for g2 in range(2):
                    pt = ps()
                    for k4 in range(4):
                        kx = 4 * g2 + k4
                        P.do("pe", "transpose", out=pt[:, k4 * 128:k4 * 128 + n], in_=x_[0:n, kx * 128:(kx + 1) * 128], identity=ident[0:n, 0:n])
                    evac(xeT[:, 4 * g2:4 * g2 + 4, t0:t0 + n], pt.re("p (a c) -> p a c", c=128)[:, :, 0:n])

        uload(0)
        uload(1)
        gather(0)
        npairs = len(useq) // 2
        for p in range(npairs):
            if p + 1 < npairs:
                uload(2 * p + 2)
                uload(2 * p + 3)
            u = useq[2 * p]
            e = u[0]
            w0 = wbf[(2 * p) % 6]
            w1 = wbf[(2 * p + 1) % 6]
            if u[1] == "g":
                fb = u[2]
                for fc in range(4):
                    f = fb * 4 + fc
                    for (s0, s1) in blocks:
                        n = s1 - s0
                        pa = ps()
                        for k in range(8):
                            mm(pa[:, 0:n], w0[:, k, fc * 128:(fc + 1) * 128], xeT[:, k, s0:s1], start=(k == 0), stop=(k == 7), r=False)
                        pu = ps()
                        for k in range(8):
                            mm(pu[:, 0:n], w1[:, k, fc * 128:(fc + 1) * 128], xeT[:, k, s0:s1], start=(k == 0), stop=(k == 7), r=False)
                        ta = tmpa[(fc + s0 // 512) % 2]
                        P.do("act", "activation", out=ta[:, 0:n], in_=pa[:, 0:n], func=AF.Silu)
                        P.do("dve", "tensor_tensor", out=gT[:, f, s0:s1], in0=ta[:, 0:n], in1=pu[:, 0:n], op=ALU.mult)
                if fb == 3 and e + 1 < NE:
                    gather(e + 1)
            else:
                dh = u[2]
                for ti, (t0, n, slot, icol, wcol) in enumerate(tiles):
                    py = ps()
                    for f in range(16):
                        mm(py[0:n, :], gT[:, f, t0:t0 + n], (w0 if f < 8 else w1)[:, f % 8, :], start=(f == 0), stop=(f == 15), r=False)
                    P.do("dve", "scalar_tensor_tensor", out=yst[0:n, ti, dh * 512:(dh + 1) * 512], in0=py[0:n, :], scalar=wcol(e)[0:n],
                         in1=g2B[slot][0:n, dh * 512:(dh + 1) * 512], op0=ALU.mult, op1=ALU.mult)
                if dh == 1:
                    for ti, (t0, n, slot, icol, wcol) in enumerate(tiles):
                        ic = icol(e)

                        def sc(eng, ic=ic, n=n, ti=ti):
                            return eng.indirect_dma_start(out=Xflat, out_offset=bass.IndirectOffsetOnAxis(ap=ic.ap, axis=0),
                                                          in_=yst.ap[0:n, ti, :], in_offset=None, compute_op=ALU.add)
                        P.dma_raw("pool", sc, list(ic.res) + list(yst.res) + [P.R(("Xs",))], [P.R(("Xs",))])
        P.barrier()

    PH = build_program.phases
    for l in range(DEPTH):
        P.mark("start")
        phase_mod(l)
        P.mark("mod")
        for b in range(NB):
            phase_proj(l, b)
            P.mark("proj")
            if "conv" in PH:
                phase_conv(l, b)
                P.mark("conv")
            if "hgrn" in PH:
                phase_hgrn(l, b)
                P.mark("hgrn")
            if "na" in PH:
                phase_na(l, b)
                P.mark("na")
            if "merge" in PH:
                phase_merge(l, b)
                P.mark("merge")
        if "moe" in PH:
            phase_moe(l)
            P.mark("moe")
        if "stop1" in PH:
            break

    for b in range(NB):
        for j in range(S // 512):
            P.dma("sp", y_out[b, j * 512:(j + 1) * 512, :], X[b, CTX + j * 512:CTX + (j + 1) * 512, :],
                  rd=[("X", b, CTX // 128 + 4 * j + i) for i in range(4)])
    P.barrier()
    P.emit()
    return nc, P


build_program.phases = ("conv", "hgrn", "na", "merge", "moe")


def make_consts():
    c = np.zeros((7, 128, 128), np.float32)
    c[0] = np.eye(128)
    s = np.arange(128)[:, None]
    t = np.arange(128)[None, :]
    c[1] = (s <= t)
    c[2] = (s >= t)
    c[3][:64, :64] = 1.0
    c[3][64:, 64:] = 1.0
    c[4] = 1.0
    kcol = np.arange(128)[:, None] % 64
    q = np.arange(64)[None, :]
    ws = np.clip(q - 8, 0, 48)
    inw = (kcol >= ws) & (kcol < ws + 16)
    c[5][:, :64] = np.where(inw, 0.0, NEG)
    return c


def expand_rpb(rpb):
    p = np.arange(128)
    wpar = (p // 64)[:, None, None]
    kcol = (p % 64)[:, None, None]
    j = np.arange(15)[None, :, None]
    q = np.arange(64)[None, None, :]
    dr = np.minimum(j + wpar, 14) + 0 * q
    dc = np.clip(kcol - q + 15, 0, 30) + 0 * j
    return np.ascontiguousarray(rpb[:, :, dr, dc])


_CACHE = {}


def run(inputs, NB, S, DEPTH, n_cores, dbg=()):
    key = (NB, S, DEPTH, tuple(dbg), build_program.phases)
    if key not in _CACHE:
        _CACHE[key] = build_program(NB, S, DEPTH, dbg)
    nc, P = _CACHE[key]
    consts = make_consts()
    rpbx = expand_rpb(np.asarray(inputs["na_rpb"], np.float32))
    shared = {k: np.ascontiguousarray(np.asarray(v, np.float32)) for k, v in inputs.items()
              if k not in ("x", "c", "ctx", "na_rpb")}
    shared["rpbx"] = rpbx
    shared["consts"] = consts
    in_maps = []
    for i in range(n_cores):
        m = dict(shared)
        m["x"] = np.ascontiguousarray(inputs["x"][i * NB:(i + 1) * NB])
        m["c"] = np.ascontiguousarray(inputs["c"][i * NB:(i + 1) * NB])
        m["ctx"] = np.ascontiguousarray(inputs["ctx"][i * NB:(i + 1) * NB])
        in_maps.append(m)
    res = run_bass_kernel_spmd(nc, in_maps, core_ids=list(range(n_cores)))
    return res


def kernel(**inputs):
    res = run(inputs, 2, 4096, 4, 8)
    return np.concatenate([r["y"] for r in res.results], axis=0).astype(np.float32)
```
